# Optimizing a Trainium2 kernel written in Bass

```python
import math
import jax, jax.numpy as jnp
from jax import lax
import numpy as np

D_MODEL = 1024
BATCH = 16
SEQ = 4096
DEPTH = 1

CTX_LEN = 256
GRID_W = 64
EPS = 1e-6
F32 = jnp.float32

HY_WIDTH = D_MODEL
HY_ORDER = 2
HY_EMB = 33
HY_BANDS = (HY_EMB - 1) // 2
HY_FFN = 64
HY_SHORT = 3
HY_FAST_DECAY = 0.3
HY_SLOW_DECAY = 1.5
HY_TARGET = 1e-2
HY_COLS = (HY_ORDER + 1) * HY_WIDTH

GLA_HEADS = 4
GLA_DK = D_MODEL // 2 // GLA_HEADS
GLA_DV = D_MODEL // GLA_HEADS
GLA_RANK = 16
GLA_TAU = 16.0
GLA_CHUNK = 64
QK_W = GLA_HEADS * GLA_DK
V_W = GLA_HEADS * GLA_DV
A_W = 2 * GLA_RANK
STATE_COLS = QK_W + V_W + A_W
GATE_COLS = 2 * D_MODEL
N_IN = STATE_COLS + QK_W + V_W + HY_COLS + GATE_COLS

N_EXPERTS = 64
N_GROUPS = 8
TOPK_GROUPS = 4
TOP_K = 8
D_EXPERT = D_MODEL // 4
D_SHARED = D_EXPERT
ROUTED_SCALE = 2.5
MOE_BLOCK = 128

kernel_name = 'hybrid_hyena_gla_moe_dit_block'


def rms_norm(x, w):
    xf = x.astype(F32)
    y = xf * lax.rsqrt(jnp.mean(xf * xf, axis=-1, keepdims=True) + EPS)
    return (y * w.astype(F32)).astype(x.dtype)


def modulate(xn, shift, scale):
    return xn * (1.0 + scale) + shift


def short_conv(u, w, b, n_rows):
    bn, L, ch = u.shape
    row_len = L // n_rows
    pad = HY_SHORT // 2
    p = jnp.pad(u.reshape(bn, n_rows, row_len, ch), ((0, 0), (0, 0), (pad, pad), (0, 0)))
    y = sum(p[:, :, j:j + row_len] * w[j] for j in range(HY_SHORT))
    return (y + b).reshape(bn, L, ch)


def hyena_filter_spectra(L, lp):
    t = jnp.linspace(0.0, 1.0, L, dtype=F32)[:, None]
    w = 2.0 * math.pi * jnp.arange(L, dtype=F32)[:, None] / L
    f = jnp.linspace(1e-4, HY_BANDS - 1, HY_BANDS, dtype=F32)[None, :]
    z = jnp.concatenate([t, jnp.cos(f * w), -jnp.sin(f * w)], axis=-1)
    freq = lp['hy_freq'].astype(F32)
    h = jnp.sin(freq[0] * (z @ lp['hy_w1'].astype(F32) + lp['hy_b1'].astype(F32)))
    h = jnp.sin(freq[1] * (h @ lp['hy_w2'].astype(F32) + lp['hy_b2'].astype(F32)))
    h = (h @ lp['hy_w3'].astype(F32)).reshape(L, 2 * HY_ORDER, HY_WIDTH)
    max_decay = math.log(HY_TARGET) / HY_FAST_DECAY
    min_decay = math.log(HY_TARGET) / HY_SLOW_DECAY
    deltas = jnp.abs(jnp.linspace(min_decay, max_decay, HY_WIDTH, dtype=F32))
    h = h * jnp.exp(-t * deltas)[:, None, :]
    hf, hb = h[:, 0::2], h[:, 1::2]
    kern = jnp.concatenate([hf, jnp.zeros_like(hf[:1]), hb[:0:-1]], axis=0)
    kern = kern * lax.rsqrt(jnp.sum(kern * kern, axis=0, keepdims=True))
    return jnp.fft.rfft(kern, axis=0)


def fft_long_conv(u, spec, bias):
    L = u.shape[1]
    uf = u.astype(F32)
    y = jnp.fft.irfft(jnp.fft.rfft(uf, n=2 * L, axis=1) * spec[None], n=2 * L, axis=1)[:, :L]
    return (y + uf * bias.astype(F32)).astype(u.dtype)


def hyena_mixer(u_hy, n_rows, lp):
    L = u_hy.shape[1]
    parts = jnp.split(short_conv(u_hy, lp['hy_conv_w'], lp['hy_conv_b'], n_rows), HY_ORDER + 1, axis=-1)
    spec = hyena_filter_spectra(L, lp)
    z = parts[0]
    for o in range(HY_ORDER):
        z = parts[o + 1] * fft_long_conv(z, spec[:, o], lp['hy_bias'][o])
    return z


def heads(a, d):
    return a.reshape(a.shape[0], a.shape[1], -1, d).transpose(0, 2, 1, 3)


def gla_inputs(u_k, u_v, u_a, lp):
    bn, L, _ = u_k.shape
    gl = jnp.einsum('blnr,nrk->nblk', u_a.reshape(bn, L, 2, GLA_RANK), lp['gla_a_w2']) + lp['gla_a_b'][:, None, None, :]
    gl = jax.nn.log_sigmoid(gl.astype(F32)) / GLA_TAU
    return heads(u_k, GLA_DK), heads(u_v, GLA_DV), heads(gl[0], GLA_DK), heads(gl[1], GLA_DK)


def gla_chunked(k, v, g, s0, exclusive, q=None):
    bn, H, L, _ = k.shape
    nc = L // GLA_CHUNK

    def chunks(a):
        return jnp.moveaxis(a.astype(F32).reshape(bn, H, nc, GLA_CHUNK, a.shape[-1]), 2, 0)

    idx = jnp.arange(GLA_CHUNK)
    mask = (idx[:, None] > idx[None, :]) if exclusive else (idx[:, None] >= idx[None, :])
    xs = (chunks(k), chunks(v), chunks(g)) if q is None else (chunks(k), chunks(v), chunks(g), chunks(q))

    def step(s, inp):
        kc, vc, gc = inp[0], inp[1], inp[2]
        b = jnp.cumsum(gc, axis=-2)
        b_last = b[..., -1:, :]
        s_new = s * jnp.exp(b_last[..., 0, :])[..., :, None] + jnp.einsum('bhck,bhcv->bhkv', kc * jnp.exp(b_last - b), vc)
        if q is None:
            return s_new, None
        qc = inp[3]
        bq = b - gc if exclusive else b
        o_inter = jnp.einsum('bhck,bhkv->bhcv', qc * jnp.exp(bq), s)
        decay = jnp.exp(jnp.where(mask[:, :, None], bq[..., :, None, :] - b[..., None, :, :], -jnp.inf))
        attn = jnp.einsum('bhik,bhjk,bhijk->bhij', qc, kc, decay)
        return s_new, o_inter + jnp.einsum('bhij,bhjv->bhiv', attn, vc)

    s_fin, o = lax.scan(step, s0, xs)
    if q is not None:
        o = jnp.moveaxis(o, 0, 2).reshape(bn, H, L, v.shape[-1])
    return o, s_fin


def gla_bidirectional(k, v, gf, gb, s0f, s0b, q=None):
    flip = lambda a: jnp.flip(a, axis=2)
    o_f, s_f = gla_chunked(k, v, gf, s0f, False, q)
    o_b, s_b = gla_chunked(flip(k), flip(v), flip(gb), s0b, True, None if q is None else flip(q))
    o = None if q is None else o_f + flip(o_b)
    return o, s_f, s_b


def token_mixer(un, n_rows, s0f, s0b, lp):
    bn, L, _ = un.shape
    u = un @ lp['w_in']
    c0 = STATE_COLS + QK_W + V_W
    u_k, u_v, u_a, u_q, u_g, u_hy, u_gate = jnp.split(
        u, [QK_W, QK_W + V_W, STATE_COLS, STATE_COLS + QK_W, c0, c0 + HY_COLS], axis=-1)
    y_hy = hyena_mixer(u_hy, n_rows, lp)
    k, v, gf, gb = gla_inputs(u_k, u_v, u_a, lp)
    q = heads(u_q, GLA_DK) * (GLA_DK ** -0.5)
    o, s_f, s_b = gla_bidirectional(k, v, gf, gb, s0f, s0b, q)
    o = o.transpose(0, 2, 1, 3).astype(un.dtype)
    o = rms_norm(o, lp['gla_norm_w'].reshape(GLA_HEADS, GLA_DV)) * jax.nn.silu(u_g).reshape(bn, L, GLA_HEADS, GLA_DV)
    y_gla = o.reshape(bn, L, V_W)
    gate_hy, gate_gla = jnp.split(jax.nn.sigmoid(u_gate), 2, axis=-1)
    merged = gate_hy * (y_hy @ lp['proj_hy']) + gate_gla * (y_gla @ lp['proj_gla'])
    return merged @ lp['w_out'], s_f, s_b


def context_states(cn, lp):
    u = cn @ lp['w_in'][:, :STATE_COLS]
    u_k, u_v, u_a = jnp.split(u, [QK_W, QK_W + V_W], axis=-1)
    k, v, gf, gb = gla_inputs(u_k, u_v, u_a, lp)
    s0 = jnp.zeros((cn.shape[0], GLA_HEADS, GLA_DK, GLA_DV), F32)
    _, s_f, s_b = gla_bidirectional(k, v, gf, gb, s0, s0)
    return s_f, s_b


def moe_ffn(h, lp):
    T = h.shape[0]
    scores = jax.nn.sigmoid((h @ lp['router_w']).astype(F32))
    sel = scores + lp['router_bias'].astype(F32)
    grp_score = lax.top_k(sel.reshape(T, N_GROUPS, N_EXPERTS // N_GROUPS), 2)[0].sum(-1)
    _, g_idx = lax.top_k(grp_score, TOPK_GROUPS)
    g_mask = jax.nn.one_hot(g_idx, N_GROUPS, dtype=F32).sum(1)
    e_mask = jnp.repeat(g_mask, N_EXPERTS // N_GROUPS, axis=-1)
    _, e_idx = lax.top_k(jnp.where(e_mask > 0, sel, -jnp.inf), TOP_K)
    w = jnp.take_along_axis(scores, e_idx, axis=-1)
    w = w / jnp.sum(w, axis=-1, keepdims=True) * ROUTED_SCALE
    comb = jnp.einsum('tk,tke->te', w, jax.nn.one_hot(e_idx, N_EXPERTS, dtype=F32)).astype(h.dtype)

    def block(args):
        hb, cb = args
        a = jax.nn.silu(jnp.einsum('td,edf->tef', hb, lp['exp_w1'])) * jnp.einsum('td,edf->tef', hb, lp['exp_w3'])
        return jnp.einsum('tef,efd->td', a * cb[:, :, None], lp['exp_w2'])

    nb = T // MOE_BLOCK
    routed = lax.map(block, (h.reshape(nb, MOE_BLOCK, -1), comb.reshape(nb, MOE_BLOCK, -1))).reshape(T, -1)
    shared = (jax.nn.silu(h @ lp['sh_w1']) * (h @ lp['sh_w3'])) @ lp['sh_w2']
    return routed + shared


def setup_inputs(seed: int = 0) -> dict:
    key = jax.random.key(seed)
    ks = jax.random.split(key, 33)
    D = D_MODEL

    def nrm(k, shape, scale):
        return scale * jax.random.normal(k, shape, F32)

    return {
        'x': nrm(ks[0], (BATCH, SEQ, D), 1.0),
        'c': nrm(ks[1], (BATCH, D), 1.0),
        'ctx': nrm(ks[2], (BATCH, CTX_LEN, D), 1.0),
        'c_ctx': nrm(ks[3], (D,), 1.0),
        'ada_w': nrm(ks[4], (DEPTH, D, 6 * D), 0.5 * D ** -0.5),
        'ada_b': nrm(ks[5], (DEPTH, 6 * D), 0.02),
        'norm1_w': 1.0 + nrm(ks[6], (DEPTH, D), 0.02),
        'norm2_w': 1.0 + nrm(ks[7], (DEPTH, D), 0.02),
        'w_in': nrm(ks[8], (DEPTH, D, N_IN), D ** -0.5),
        'hy_conv_w': nrm(ks[9], (DEPTH, HY_SHORT, HY_COLS), HY_SHORT ** -0.5),
        'hy_conv_b': nrm(ks[10], (DEPTH, HY_COLS), 0.02),
        'hy_w1': nrm(ks[11], (DEPTH, HY_EMB, HY_FFN), HY_EMB ** -0.5),
        'hy_b1': nrm(ks[12], (DEPTH, HY_FFN), 0.1),
        'hy_freq': 1.0 + nrm(ks[13], (DEPTH, 2, HY_FFN), 0.02),
        'hy_w2': nrm(ks[14], (DEPTH, HY_FFN, HY_FFN), HY_FFN ** -0.5),
        'hy_b2': nrm(ks[15], (DEPTH, HY_FFN), 0.1),
        'hy_w3': nrm(ks[16], (DEPTH, HY_FFN, 2 * HY_ORDER * HY_WIDTH), HY_FFN ** -0.5),
        'hy_bias': nrm(ks[17], (DEPTH, HY_ORDER, HY_WIDTH), 0.5),
        'gla_a_w2': nrm(ks[18], (DEPTH, 2, GLA_RANK, QK_W), GLA_RANK ** -0.5),
        'gla_a_b': nrm(ks[19], (DEPTH, 2, QK_W), 0.1),
        'gla_norm_w': 1.0 + nrm(ks[20], (DEPTH, V_W), 0.02),
        'proj_hy': nrm(ks[21], (DEPTH, HY_WIDTH, D), HY_WIDTH ** -0.5),
        'proj_gla': nrm(ks[22], (DEPTH, V_W, D), V_W ** -0.5),
        'w_out': nrm(ks[23], (DEPTH, D, D), D ** -0.5),
        'router_w': nrm(ks[24], (DEPTH, D, N_EXPERTS), D ** -0.5),
        'router_bias': nrm(ks[25], (DEPTH, N_EXPERTS), 0.01),
        'exp_w1': nrm(ks[26], (DEPTH, N_EXPERTS, D, D_EXPERT), D ** -0.5),
        'exp_w3': nrm(ks[27], (DEPTH, N_EXPERTS, D, D_EXPERT), D ** -0.5),
        'exp_w2': nrm(ks[28], (DEPTH, N_EXPERTS, D_EXPERT, D), D_EXPERT ** -0.5),
        'sh_w1': nrm(ks[29], (DEPTH, D, D_SHARED), D ** -0.5),
        'sh_w3': nrm(ks[30], (DEPTH, D, D_SHARED), D ** -0.5),
        'sh_w2': nrm(ks[31], (DEPTH, D_SHARED, D), D_SHARED ** -0.5),
        'final_norm_w': 1.0 + nrm(ks[32], (D,), 0.02),
    }


def reference(x, c, ctx, c_ctx, ada_w, ada_b, norm1_w, norm2_w, w_in, hy_conv_w, hy_conv_b,
              hy_w1, hy_b1, hy_freq, hy_w2, hy_b2, hy_w3, hy_bias, gla_a_w2, gla_a_b, gla_norm_w,
              proj_hy, proj_gla, w_out, router_w, router_bias, exp_w1, exp_w3, exp_w2,
              sh_w1, sh_w3, sh_w2, final_norm_w):
    n_rows = x.shape[1] // GRID_W
    for i in range(DEPTH):
        last = i == DEPTH - 1
        lp = {
            'w_in': w_in[i], 'hy_conv_w': hy_conv_w[i], 'hy_conv_b': hy_conv_b[i],
            'hy_w1': hy_w1[i], 'hy_b1': hy_b1[i], 'hy_freq': hy_freq[i], 'hy_w2': hy_w2[i],
            'hy_b2': hy_b2[i], 'hy_w3': hy_w3[i], 'hy_bias': hy_bias[i],
            'gla_a_w2': gla_a_w2[i], 'gla_a_b': gla_a_b[i], 'gla_norm_w': gla_norm_w[i],
            'proj_hy': proj_hy[i], 'proj_gla': proj_gla[i], 'w_out': w_out[i],
            'router_w': router_w[i], 'router_bias': router_bias[i],
            'exp_w1': exp_w1[i], 'exp_w3': exp_w3[i], 'exp_w2': exp_w2[i],
            'sh_w1': sh_w1[i], 'sh_w3': sh_w3[i], 'sh_w2': sh_w2[i],
        }
        sh1, sc1, g1, sh2, sc2, g2 = [m[:, None, :] for m in jnp.split(jax.nn.silu(c) @ ada_w[i] + ada_b[i], 6, axis=-1)]
        csh1, csc1, cg1, csh2, csc2, cg2 = jnp.split(jax.nn.silu(c_ctx) @ ada_w[i] + ada_b[i], 6, axis=-1)

        cn = modulate(rms_norm(ctx, norm1_w[i]), csh1, csc1)
        if last:
            s_f, s_b = context_states(cn, lp)
        else:
            s0 = jnp.zeros((ctx.shape[0], GLA_HEADS, GLA_DK, GLA_DV), F32)
            y_c, s_f, s_b = token_mixer(cn, 1, s0, s0, lp)
        xn = modulate(rms_norm(x, norm1_w[i]), sh1, sc1)
        y_x, _, _ = token_mixer(xn, n_rows, s_f, s_b, lp)
        x = x + g1 * y_x

        xn2 = modulate(rms_norm(x, norm2_w[i]), sh2, sc2)
        x = x + g2 * moe_ffn(xn2.reshape(-1, D_MODEL), lp).reshape(x.shape)
        if not last:
            ctx = ctx + cg1 * y_c
            cn2 = modulate(rms_norm(ctx, norm2_w[i]), csh2, csc2)
            ctx = ctx + cg2 * moe_ffn(cn2.reshape(-1, D_MODEL), lp).reshape(ctx.shape)
    return rms_norm(x, final_norm_w)
```

```python
import math
import numpy as np
import concourse.bass as bass
import concourse.mybir as mybir
from concourse.bass_utils import run_bass_kernel_spmd
from contextlib import ExitStack

F32 = mybir.dt.float32
BF16 = mybir.dt.bfloat16
I32 = mybir.dt.int32
ALU = mybir.AluOpType
AF = mybir.ActivationFunctionType
AX = mybir.AxisListType

ENGS = ("pe", "act", "dve", "pool", "sp")
N_DMA_SEMS = 48
SAME_ENGINE_NOSYNC = ("pe",)

D = 1024
L = 4096
CTX = 256
NB = 2
N_IN = 8224
EPS = 1e-6
C_K, C_V, C_A, C_Q, C_G, C_HY, C_GATE = 0, 512, 1536, 1568, 2080, 3104, 6176


class Prog:
    def __init__(self, nc):
        self.nc = nc
        self.es = ExitStack()
        self.stack = [self.es]
        self.streams = {e: [] for e in ENGS}
        self.cnt = {e: 0 for e in ENGS}
        self.esem = {}
        for e in ENGS:
            self.esem[e] = self.es.enter_context(nc.semaphore("sem_" + e))
        self.dsem = [self.es.enter_context(nc.semaphore("dsem%d" % i)) for i in range(N_DMA_SEMS)]
        self.dcnt = [0] * N_DMA_SEMS
        self.dnext = 0
        self.waited = {e: {} for e in ENGS}
        self.last_w = {}
        self.readers = {}
        self.n_ops = 0
        self.uid = 0

    def push(self):
        es = ExitStack()
        self.stack.append(es)

    def pop(self):
        self.barrier()
        self.stack.pop().close()

    def sbuf(self, name, shape, dtype):
        self.uid += 1
        return self.stack[-1].enter_context(self.nc.sbuf_tensor("%s_%d" % (name, self.uid), list(shape), dtype))

    def psum(self, name, shape, dtype):
        return self.stack[-1].enter_context(self.nc.psum_tensor(name, list(shape), dtype))

    def _deps(self, reads, writes):
        deps = []
        for k in reads:
            t = self.last_w.get(k)
            if t is not None:
                deps.append(t)
        for k in writes:
            t = self.last_w.get(k)
            if t is not None:
                deps.append(t)
            deps.extend(self.readers.get(k, ()))
        return deps

    def _update(self, tok, reads, writes):
        for k in reads:
            self.readers.setdefault(k, []).append(tok)
        for k in writes:
            self.last_w[k] = tok
            self.readers[k] = []

    def _waits(self, eng, deps):
        need = {}
        own = "e_" + eng
        for (s, v, sid) in deps:
            if sid == own and eng in SAME_ENGINE_NOSYNC:
                continue
            if need.get(sid, (None, 0))[1] < v:
                need[sid] = (s, v)
        out = []
        w = self.waited[eng]
        for sid, (s, v) in need.items():
            if w.get(sid, 0) >= v:
                continue
            w[sid] = v
            out.append((s, v))
        return out

    def op(self, eng, fn, reads=(), writes=(), extra=()):
        deps = self._deps(reads, writes) + list(extra)
        waits = self._waits(eng, deps)
        self.cnt[eng] += 1
        tok = (self.esem[eng], self.cnt[eng], "e_" + eng)
        self.streams[eng].append((waits, fn, (self.esem[eng], 1)))
        self._update(tok, reads, writes)
        self.n_ops += 1
        return tok

    def dma(self, eng, out, in_, reads=(), writes=(), extra=(), **kw):
        i = self.dnext
        self.dnext = (self.dnext + 1) % N_DMA_SEMS
        deps = self._deps(reads, writes) + list(extra)
        if self.dcnt[i] > 0:
            deps.append((self.dsem[i], self.dcnt[i], "d%d" % i))
        waits = self._waits(eng, deps)
        self.dcnt[i] += 16
        tok = (self.dsem[i], self.dcnt[i], "d%d" % i)

        def fn(e, out=out, in_=in_, kw=kw):
            return e.dma_start(out=out, in_=in_, **kw)

        self.streams[eng].append((waits, fn, (self.dsem[i], 16)))
        self._update(tok, reads, writes)
        self.n_ops += 1
        return tok

    def barrier(self):
        toks = [(self.esem[e], self.cnt[e], "e_" + e) for e in ENGS if self.cnt[e] > 0]
        toks += [(self.dsem[i], self.dcnt[i], "d%d" % i) for i in range(N_DMA_SEMS) if self.dcnt[i] > 0]
        for e in ENGS:
            need = {}
            for (s, v, sid) in toks:
                need[sid] = (s, v)
            w = self.waited[e]
            waits = []
            for sid, (s, v) in need.items():
                if sid == "e_" + e:
                    continue
                if w.get(sid, 0) >= v:
                    continue
                w[sid] = v
                waits.append((s, v))
            if waits:
                self.streams[e].append((waits, None, None))
        self.last_w.clear()
        self.readers.clear()

    def emit(self):
        nc = self.nc
        streams = self.streams

        def run(e, name):
            for (waits, fn, inc) in streams[name]:
                for (s, v) in waits:
                    e.wait_ge(s, v)
                if fn is not None:
                    ins = fn(e)
                    if inc is not None:
                        ins.then_inc(inc[0], inc[1])

        with nc.Block() as block:
            @block.tensor
            def _(e):
                run(e, "pe")

            @block.scalar
            def _(e):
                run(e, "act")

            @block.vector
            def _(e):
                run(e, "dve")

            @block.gpsimd
            def _(e):
                run(e, "pool")

            @block.sync
            def _(e):
                run(e, "sp")

    def close(self):
        self.es.close()


def host_consts():
    c = {}
    c["ident"] = np.eye(128, dtype=np.float32)
    j = np.arange(128)[:, None]
    i = np.arange(128)[None, :]
    c["maskf"] = (j <= i).astype(np.float32)
    c["maskb"] = (j > i).astype(np.float32)
    sel = np.zeros((64, 64, 128), np.float32)
    for e in range(64):
        sel[e, e, :] = 1.0
    c["sel"] = sel
    Lh = L
    t = np.linspace(0.0, 1.0, Lh, dtype=np.float32)
    w = (2.0 * math.pi * np.arange(Lh, dtype=np.float32) / Lh).astype(np.float32)
    f = np.linspace(1e-4, 15.0, 16, dtype=np.float32)
    z = np.concatenate([t[:, None], np.cos(f[None, :] * w[:, None]), -np.sin(f[None, :] * w[:, None])], axis=1).astype(np.float32)
    zext = np.zeros((2 * Lh, 33), np.float32)
    zext[:Lh] = z
    zext[Lh + 1:] = z[:0:-1]
    text = np.zeros((2 * Lh,), np.float32)
    text[:Lh] = t
    text[Lh] = 1.0e4
    text[Lh + 1:] = t[:0:-1]
    c["zext"] = np.ascontiguousarray(zext.T)
    c["text"] = text.reshape(1, -1)
    max_decay = math.log(1e-2) / 0.3
    min_decay = math.log(1e-2) / 1.5
    deltas = np.abs(np.linspace(min_decay, max_decay, 1024, dtype=np.float32))
    c["deltaT"] = np.ascontiguousarray(deltas.reshape(8, 128).T)
    N = 8192
    n1 = np.arange(64, dtype=np.float64)[:, None]
    k1 = np.arange(64, dtype=np.float64)[None, :]
    ang = 2 * np.pi * n1 * k1 / 64
    c["tF64"] = np.concatenate([np.cos(ang), -np.sin(ang), np.sin(ang), np.cos(ang)], axis=1).astype(np.float32)
    n2 = np.arange(128, dtype=np.float64)[:, None, None]
    k1 = np.arange(64, dtype=np.float64)[None, :, None]
    k2 = np.arange(128, dtype=np.float64)[None, None, :]
    ang = 2 * np.pi * n2 * (k1 + 64 * k2) / N
    c["tG"] = np.stack([np.cos(ang), -np.sin(ang)], axis=2).astype(np.float32)
    kl = np.arange(64, dtype=np.float64)[:, None, None]
    ta = np.arange(128, dtype=np.float64)[None, :, None]
    tb = np.arange(32, dtype=np.float64)[None, None, :]
    ang = 2 * np.pi * (ta + 128 * tb) * kl / N
    c["tGp"] = (np.concatenate([np.cos(ang), -np.sin(ang)], axis=0) / N).astype(np.float32)
    kh = np.arange(128, dtype=np.float64)[:, None]
    ta = np.arange(128, dtype=np.float64)[None, :]
    ang = 2 * np.pi * ta * kh / 128
    c["tC3"] = np.stack([np.cos(ang), np.sin(ang), -np.sin(ang)], axis=1).astype(np.float32)
    return c


def hc_a_w2e(a_w2):
    o = np.zeros((32, 2, 512), np.float32)
    o[0:16, 0, :] = a_w2[0]
    o[16:32, 1, :] = a_w2[1]
    return o


def fm(v, nchunk):
    return np.ascontiguousarray(np.asarray(v, np.float32).reshape(nchunk, 128).T)


class K:
    pass


def build_program(dbg=()):
    nc = bass.Bass("TRN2", target_bir_lowering=False)
    dbg = set(dbg)

    def din(name, shape, dt=F32):
        return nc.dram_tensor(name, list(shape), dt, kind="ExternalInput").ap()

    def dscr(name, shape, dt=F32):
        kind = "ExternalOutput" if name in dbg else "Internal"
        return nc.dram_tensor(name, list(shape), dt, kind=kind).ap()

    g = K()
    g.nc = nc
    g.x = din("x", [NB, L, D])
    g.ctx = din("ctx", [NB, CTX, D])
    g.c3T = din("c3T", [128, 8, 4])
    g.ada_w = din("ada_w", [D, 6 * D])
    g.ada_bT = din("ada_bT", [128, 48])
    g.ada_b = din("ada_b", [1, 6 * D])
    g.norm1_wT = din("norm1_wT", [128, 8])
    g.norm2_wT = din("norm2_wT", [128, 8])
    g.w_in = din("w_in", [D, N_IN])
    g.hy_conv_wT = din("hy_conv_wT", [128, 24, 3])
    g.hy_conv_bT = din("hy_conv_bT", [128, 24])
    g.ident = din("ident", [128, 128])
    g.a_w2e = din("a_w2e", [32, 2, 512])
    g.a_bT = din("a_bT", [128, 2, 4])
    g.gnwT = din("gnwT", [128, 4, 2])
    g.maskf = din("maskf", [128, 128])
    g.maskb = din("maskb", [128, 128])
    g.proj_hy = din("proj_hy", [D, D])
    g.proj_gla = din("proj_gla", [D, D])
    g.w_out = din("w_out", [D, D])
    g.router_w = din("router_w", [D, 64])
    g.router_b = din("router_b", [1, 64])
    g.final_w = din("final_w", [1, D])
    g.exp_w1 = din("exp_w1", [64, D, 256])
    g.exp_w3 = din("exp_w3", [64, D, 256])
    g.exp_w2 = din("exp_w2", [64, 256, D])
    g.sh_w1 = din("sh_w1", [D, 256])
    g.sh_w3 = din("sh_w3", [D, 256])
    g.sh_w2 = din("sh_w2", [256, D])
    g.sel = din("sel", [64, 64, 128])
    g.zext = din("zext", [33, 8192])
    g.text = din("text", [1, 8192])
    g.deltaT = din("deltaT", [128, 8])
    g.hy_w1 = din("hy_w1", [33, 64])
    g.hy_w2 = din("hy_w2", [64, 64])
    g.hy_w3 = din("hy_w3", [64, 4096])
    g.hy_bf = din("hy_bf", [64, 4])
    g.hy_biasT = din("hy_biasT", [64, 16, 2])
    g.tF64 = din("tF64", [64, 256])
    g.tG = din("tG", [128, 64, 2, 128])
    g.tGp = din("tGp", [128, 128, 32])
    g.tC3 = din("tC3", [128, 3, 128])
    g.out = nc.dram_tensor("out", [NB, L, D], F32, kind="ExternalOutput").ap()
    g.uhy = [dscr("uhy%d" % b, [3, D, L], BF16) for b in range(NB)]
    g.gate = [dscr("gate%d" % b, [2 * D, L], BF16) for b in range(NB)]
    g.sg = [dscr("sg%d" % b, [D, L], BF16) for b in range(NB)]
    g.kT = [dscr("kT%d" % b, [512, L], BF16) for b in range(NB)]
    g.qT = [dscr("qT%d" % b, [512, L], BF16) for b in range(NB)]
    g.uaT = [dscr("uaT%d" % b, [32, L], F32) for b in range(NB)]
    g.v = [dscr("v%d" % b, [L, D], BF16) for b in range(NB)]
    g.ckT = [dscr("ckT%d" % b, [512, CTX], BF16) for b in range(NB)]
    g.cuaT = [dscr("cuaT%d" % b, [32, CTX], F32) for b in range(NB)]
    g.cv = [dscr("cv%d" % b, [CTX, D], BF16) for b in range(NB)]
    g.yglaT = [dscr("yglaT%d" % b, [D, L], BF16) for b in range(NB)]
    g.kernT = [dscr("kernT%d" % o, [D, 8192], BF16) for o in range(2)]
    g.sspec = [dscr("sspec%d" % o, [16, 128, 8192], BF16) for o in range(2)]
    g.z1T = [dscr("z1T%d" % b, [D, L], BF16) for b in range(NB)]
    g.yhyT = [dscr("yhyT%d" % b, [D, L], BF16) for b in range(NB)]
    g.x1d = [dscr("x1d%d" % b, [L, D], F32) for b in range(NB)]
    g.xn2T = [dscr("xn2T%d" % b, [D, L], BF16) for b in range(NB)]
    g.combT = [dscr("combT%d" % b, [64, L], BF16) for b in range(NB)]
    g.dbg_sc = dscr("dbg_sc", [L, 64], F32)
    g.gbc_d = dscr("gbc_d", [128, 4 * D])
    g.dbg_state = dscr("dbg_state", [128, NB * 2 * 4 * 256])
    g.dbg_h2 = dscr("dbg_h2", [64, 8192], BF16)
    g.dbg_h1 = dscr("dbg_h1", [64, 4096], F32)
    g.dbg_mt = dscr("dbg_mt", [64, 4096], F32)
    g.dbg_kern = dscr("dbg_kern", [128, 8192], F32)
    g.dbg_st = dscr("dbg_st", [128, 4], F32)
    g.dbg_modT = dscr("dbg_modT", [128, 48 * 4])
    g.dbg_gb = dscr("dbg_gb", [128, 4 * D])
    g.dbg_xnT = dscr("dbg_xnT", [128, 8 * L], BF16)

    p = Prog(nc)
    g.p = p
    g.ident_f = p.sbuf("ident_f", [128, 128], F32)
    g.ident_b = p.sbuf("ident_b", [128, 128], BF16)
    g.modT = p.sbuf("modT", [128, 48, 4], F32)
    g.scale1T = p.sbuf("scale1T", [128, 8, 4], F32)
    g.scale2T = p.sbuf("scale2T", [128, 8, 4], F32)
    g.ps = [p.psum("ps%d" % i, [128, 512], F32) for i in range(8)]

    p.dma("sp", g.ident_f[:], g.ident, writes=["ident_f"])
    p.op("dve", lambda e: e.tensor_copy(out=g.ident_b[:], in_=g.ident_f[:]), reads=["ident_f"], writes=["ident_b"])

    phase0(g)
    if "stop0" in dbg:
        p.dma("sp", g.dbg_modT, g.modT[:].rearrange("p a b -> p (a b)"), reads=["modT"], writes=["o1"])
    else:
        p.push()
        g.s0 = p.sbuf("s0", [128, NB, 2, 4, 256], F32)
        for b in range(NB):
            if "noctx" in dbg:
                break
            phase_proj(g, b, ctx=True)
            phase_gla(g, b, ctx=True)
        if "stop1" in dbg:
            p.dma("sp", g.dbg_state, g.s0[:].rearrange("p a b c d -> p (a b c d)"), reads=["s0"], writes=["o3"])
        for b in range(NB):
            if "stop1" in dbg:
                break
            if "noproj" not in dbg:
                phase_proj(g, b, ctx=False)
            if "stop3" in dbg:
                break
            if "nogla" not in dbg:
                phase_gla(g, b, ctx=False)
            if "stop4" in dbg:
                break
        p.pop()
        if "stop1" not in dbg and "stop3" not in dbg and "stop4" not in dbg:
            if "nohy" not in dbg:
                phase_filter(g)
                if "stop5" not in dbg:
                    phase_hyena(g, dbg)
            if not ({"stop5", "stop6", "stop7"} & dbg):
                for b in range(NB):
                    if "nomerge" not in dbg:
                        phase_merge(g, b, dbg)
                    if "stop8" in dbg:
                        break
                    phase_moe(g, b, dbg)
                    if "stop9" in dbg:
                        break
    p.barrier()
    p.emit()
    p.close()
    return nc


def phase0(g):
    p = g.p
    p.push()
    c3T = p.sbuf("c3T", [128, 8, 4], F32)
    sT = p.sbuf("sT", [128, 8, 4], F32)
    sTrep = p.sbuf("sTrep", [128, 8, 2, 128], F32)
    ada_bT = p.sbuf("ada_bT", [128, 48], F32)
    n1 = p.sbuf("n1", [128, 8], F32)
    n2 = p.sbuf("n2", [128, 8], F32)
    abb = p.sbuf("abb", [128, 2, D], F32)
    aw = [p.sbuf("aw%d" % i, [128, 8, 512], F32) for i in range(2)]
    g.gbc = p.sbuf("gbc", [128, 4, D], F32)
    p.dma("sp", c3T[:], g.c3T, writes=["c3T"])
    p.dma("sp", ada_bT[:], g.ada_bT, writes=["ada_bT"])
    p.dma("sp", n1[:], g.norm1_wT, writes=["n1"])
    p.dma("sp", n2[:], g.norm2_wT, writes=["n2"])
    p.dma("sp", abb[:, 0, :], g.ada_b[0:1, 2 * D:3 * D].partition_broadcast(128), writes=["abb0"])
    p.dma("sp", abb[:, 1, :], g.ada_b[0:1, 5 * D:6 * D].partition_broadcast(128), writes=["abb1"])
    p.op("act", lambda e: e.activation(out=sT[:], in_=c3T[:], func=AF.Silu), reads=["c3T"], writes=["sT"])
    for b in range(2):
        for k in range(8):
            p.op("dve", lambda e, b=b, k=k: e.tensor_copy(out=sTrep[:, k, b, :], in_=sT[:, k, b:b + 1].to_broadcast([128, 128])),
                 reads=["sT"], writes=["sTrep"])
    psT = g.ps[0]
    aw_view = g.ada_w.rearrange("(k p) n -> p k n", p=128)
    for ci in range(12):
        a = aw[ci % 2]
        ak = "aw%d" % (ci % 2)
        p.dma("sp", a[:], aw_view[:, :, ci * 512:(ci + 1) * 512], writes=[ak])
        for jj in range(4):
            j = ci * 4 + jj
            for k in range(8):
                p.op("pe", lambda e, a=a, jj=jj, j=j, k=k: e.matmul(psT[:, j * 4:(j + 1) * 4], lhsT=a[:, k, jj * 128:(jj + 1) * 128],
                                                                    rhs=sT[:, k, :], start=(k == 0), stop=(k == 7)),
                     reads=[ak, "sT"], writes=["ps0"])
        if ci in (4, 5, 10, 11):
            which = 0 if ci < 6 else 1
            half = ci % 2 if ci < 6 else (ci - 10)
            for b in range(2):
                pb = g.ps[1 + b]
                for k in range(8):
                    p.op("pe", lambda e, a=a, b=b, k=k, pb=pb: e.matmul(pb[:], lhsT=sTrep[:, k, b, :], rhs=a[:, k, :], start=(k == 0), stop=(k == 7)),
                         reads=[ak, "sTrep"], writes=["ps%d" % (1 + b)])
                p.op("dve", lambda e, b=b, which=which, half=half, pb=pb: e.tensor_tensor(
                    out=g.gbc[:, which * 2 + b, half * 512:(half + 1) * 512], in0=pb[:], in1=abb[:, which, half * 512:(half + 1) * 512], op=ALU.add),
                    reads=["ps%d" % (1 + b), "abb%d" % which], writes=["gbc"])
    p.op("dve", lambda e: e.tensor_tensor(out=g.modT[:], in0=psT[:, 0:192].rearrange("p (a b) -> p a b", b=4),
                                          in1=ada_bT[:].unsqueeze(2).to_broadcast([128, 48, 4]), op=ALU.add),
         reads=["ps0", "ada_bT"], writes=["modT"])
    p.op("dve", lambda e: e.scalar_tensor_tensor(out=g.scale1T[:], in0=g.modT[:, 8:16, :], scalar=1.0,
                                                 in1=n1[:].unsqueeze(2).to_broadcast([128, 8, 4]), op0=ALU.add, op1=ALU.mult),
         reads=["modT", "n1"], writes=["scale1T"])
    p.op("dve", lambda e: e.scalar_tensor_tensor(out=g.scale2T[:], in0=g.modT[:, 32:40, :], scalar=1.0,
                                                 in1=n2[:].unsqueeze(2).to_broadcast([128, 8, 4]), op0=ALU.add, op1=ALU.mult),
         reads=["modT", "n2"], writes=["scale2T"])
    p.dma("sp", g.gbc_d, g.gbc[:].rearrange("p a b -> p (a b)"), reads=["gbc"], writes=["gbc_d"])
    p.pop()


def norm_transpose(g, src, T, r, xnT, scaleT, shift_j0, xin, xsb, junk, stat, tagbase):
    p = g.p
    NTL = T // 128

    def st1(t):
        s = t % 2
        xi, xs = xin[s], xsb[s]
        sk = "stat%d" % s
        p.dma("sp", xi[:], src[t * 128:(t + 1) * 128, :], writes=["xin%d" % s])
        p.op("act", lambda e: e.activation(out=junk[:], in_=xi[:], func=AF.Square, accum_out=stat[:, 4 * s:4 * s + 1]),
             reads=["xin%d" % s], writes=["junk", sk])
        p.op("act", lambda e: e.activation(out=stat[:, 4 * s + 1:4 * s + 2], in_=stat[:, 4 * s:4 * s + 1], func=AF.Ln, scale=1.0 / D, bias=EPS), reads=[sk], writes=[sk])
        p.op("act", lambda e: e.activation(out=stat[:, 4 * s + 2:4 * s + 3], in_=stat[:, 4 * s + 1:4 * s + 2], func=AF.Exp, scale=-0.5), reads=[sk], writes=[sk])
        p.op("dve", lambda e: e.tensor_scalar(out=xs[:], in0=xi[:], scalar1=stat[:, 4 * s + 2:4 * s + 3], scalar2=None, op0=ALU.mult),
             reads=["xin%d" % s, sk], writes=["xsb%d" % s])

    def st2(t):
        s = t % 2
        xs = xsb[s]
        pst = g.ps[6 + s]
        pk = "ps%d" % (6 + s)
        pv = pst[:].bitcast(BF16)
        for k in range(8):
            p.op("pe", lambda e, k=k: e.transpose(pv[:, k * 128:(k + 1) * 128], xs[:, k * 128:(k + 1) * 128], g.ident_b[:]),
                 reads=["xsb%d" % s, "ident_b"], writes=[pk])
        for k in range(8):
            p.op("act", lambda e, k=k: e.activation(out=xnT[:, k, t * 128:(t + 1) * 128], in_=pv[:, k * 128:(k + 1) * 128],
                                                    func=AF.Identity, scale=scaleT[:, k, r:r + 1],
                                                    bias=g.modT[:, shift_j0 + k, r:r + 1]),
                 reads=[pk, "modT", "scale"], writes=[tagbase])

    st1(0)
    for t in range(NTL):
        if t + 1 < NTL:
            st1(t + 1)
        st2(t)


def phase_proj(g, b, ctx):
    p = g.p
    T = CTX if ctx else L
    r = 2 if ctx else b
    src = g.ctx[b] if ctx else g.x[b]
    p.push()
    xnT = p.sbuf("xnT", [128, 8, T], BF16)
    xin = [p.sbuf("xin%d" % i, [128, D], F32) for i in range(2)]
    xsb = [p.sbuf("xsb%d" % i, [128, D], BF16) for i in range(2)]
    junk = p.sbuf("junk", [128, D], F32)
    stat = p.sbuf("stat", [128, 8], F32)
    wst = [p.sbuf("wst%d" % i, [128, 8, 128], F32) for i in range(2)]
    wbf = [p.sbuf("wbf%d" % i, [128, 8, 128], BF16) for i in range(2)]
    wvst = [p.sbuf("wvst%d" % i, [128, 8, 512], F32) for i in range(2)]
    wvbf = [p.sbuf("wvbf%d" % i, [128, 8, 512], BF16) for i in range(2)]
    ost = [p.sbuf("ost%d" % i, [128, 512], BF16) for i in range(4)]
    osf = [p.sbuf("osf%d" % i, [128, 512], F32) for i in range(2)]
    cw = p.sbuf("cw", [128, 24, 3], F32)
    cb = p.sbuf("cb", [128, 24], F32)
    p.dma("sp", cw[:], g.hy_conv_wT, writes=["cw"])
    p.dma("sp", cb[:], g.hy_conv_bT, writes=["cb"])
    norm_transpose(g, src, T, r, xnT, g.scale1T, 0, xin, xsb, junk, stat, "xnT")
    if (not ctx) and b == 0:
        p.dma("act", g.dbg_xnT, xnT[:].rearrange("p a b -> p (a b)"), reads=["xnT"], writes=["dbgx"])

    TG = 512 if T >= 512 else T
    ntg = T // TG
    w_view = g.w_in.rearrange("(k p) n -> p k n", p=128)
    if ctx:
        chunks = [("k", C_K + i * 128, 128, i) for i in range(4)] + [("a", C_A, 32, 0)]
    else:
        chunks = ([("k", C_K + i * 128, 128, i) for i in range(4)] + [("a", C_A, 32, 0)] +
                  [("q", C_Q + i * 128, 128, i) for i in range(4)] +
                  [("g", C_G + i * 128, 128, i) for i in range(8)] +
                  [("hy", C_HY + i * 128, 128, i) for i in range(24)] +
                  [("gate", C_GATE + i * 128, 128, i) for i in range(16)])
    if not ctx:
        hy = [c for c in chunks if c[0] == "hy"]
        oth = [c for c in chunks if c[0] != "hy"]
        chunks = []
        while hy or oth:
            if oth:
                chunks.append(oth.pop(0))
            if hy:
                chunks.append(hy.pop(0))
    n_ep = 0
    for ci, (kind, c0, M, idx) in enumerate(chunks):
        s = ci % 2
        ws_, wb_ = wst[s], wbf[s]
        p.dma("sp", ws_[:, :, 0:M], w_view[:, :, c0:c0 + M], writes=["wst%d" % s])
        p.op("pool", lambda e, ws_=ws_, wb_=wb_, M=M: e.tensor_copy(out=wb_[:, :, 0:M], in_=ws_[:, :, 0:M]),
             reads=["wst%d" % s], writes=["wbf%d" % s])
        for tg in range(ntg):
            bank = (ci * ntg + tg) % 4
            ps = g.ps[bank]
            pk = "ps%d" % bank
            ts = slice(tg * TG, (tg + 1) * TG)
            for k in range(8):
                p.op("pe", lambda e, ps=ps, wb_=wb_, M=M, k=k, ts=ts: e.matmul(ps[0:M, 0:TG], lhsT=wb_[:, k, 0:M], rhs=xnT[:, k, ts],
                                                                              start=(k == 0), stop=(k == 7)),
                     reads=["wbf%d" % s, "xnT"], writes=[pk])
            n_ep += 1
            oi = n_ep % 4
            o = ost[oi]
            ok = "ost%d" % oi
            if kind in ("k", "q"):
                dst = (g.ckT[b] if ctx else g.kT[b]) if kind == "k" else g.qT[b]
                sc = 1.0 if kind == "k" else 128 ** -0.5
                p.op("act", lambda e, o=o, ps=ps, sc=sc: e.activation(out=o[:, 0:TG], in_=ps[:, 0:TG], func=AF.Copy, scale=sc), reads=[pk], writes=[ok])
                p.dma("act", dst[idx * 128:(idx + 1) * 128, ts], o[:, 0:TG], reads=[ok], writes=["scr"])
            elif kind == "a":
                of = osf[n_ep % 2]
                ofk = "osf%d" % (n_ep % 2)
                p.op("dve", lambda e, of=of, ps=ps: e.tensor_copy(out=of[0:32, 0:TG], in_=ps[0:32, 0:TG]), reads=[pk], writes=[ofk])
                dst = g.cuaT[b] if ctx else g.uaT[b]
                p.dma("act", dst[:, ts], of[0:32, 0:TG], reads=[ofk], writes=["scr"])
            elif kind == "g":
                p.op("act", lambda e, o=o, ps=ps: e.activation(out=o[:], in_=ps[:], func=AF.Silu), reads=[pk], writes=[ok])
                p.dma("act", g.sg[b][idx * 128:(idx + 1) * 128, ts], o[:], reads=[ok], writes=["scr"])
            elif kind == "gate":
                p.op("act", lambda e, o=o, ps=ps: e.activation(out=o[:], in_=ps[:], func=AF.Sigmoid), reads=[pk], writes=[ok])
                p.dma("act", g.gate[b][idx * 128:(idx + 1) * 128, ts], o[:], reads=[ok], writes=["scr"])
            elif kind == "hy":
                of = osf[n_ep % 2]
                ofk = "osf%d" % (n_ep % 2)
                p.op("act", lambda e, of=of, ps=ps, idx=idx: e.activation(out=of[:], in_=ps[:], func=AF.Identity, scale=cw[:, idx, 1:2], bias=cb[:, idx:idx + 1]),
                     reads=[pk, "cw", "cb"], writes=[ofk])
                ofv = of[:].rearrange("p (r c) -> p r c", c=64)
                psv = ps[:].rearrange("p (r c) -> p r c", c=64)
                p.op("dve", lambda e, ofv=ofv, psv=psv, idx=idx: e.scalar_tensor_tensor(out=ofv[:, :, 1:64], in0=psv[:, :, 0:63], scalar=cw[:, idx, 0:1],
                                                                                    in1=ofv[:, :, 1:64], op0=ALU.mult, op1=ALU.add),
                     reads=[pk, ofk, "cw"], writes=[ofk])
                p.op("dve", lambda e, o=o, ofv=ofv, psv=psv, idx=idx: e.scalar_tensor_tensor(
                    out=o[:].rearrange("p (r c) -> p r c", c=64)[:, :, 0:63], in0=psv[:, :, 1:64], scalar=cw[:, idx, 2:3],
                    in1=ofv[:, :, 0:63], op0=ALU.mult, op1=ALU.add),
                    reads=[pk, ofk, "cw"], writes=[ok])
                p.op("dve", lambda e, o=o, ofv=ofv: e.tensor_copy(out=o[:].rearrange("p (r c) -> p r c", c=64)[:, :, 63:64], in_=ofv[:, :, 63:64]),
                     reads=[ofk], writes=[ok])
                part, cc = idx // 8, idx % 8
                p.dma("act", g.uhy[b][part, cc * 128:(cc + 1) * 128, ts], o[:], reads=[ok], writes=["scr"])
    for vc in range(2):
        s = vc % 2
        p.dma("sp", wvst[s][:], w_view[:, :, C_V + vc * 512:C_V + (vc + 1) * 512], writes=["wvst%d" % s])
        p.op("pool", lambda e, s=s: e.tensor_copy(out=wvbf[s][:], in_=wvst[s][:]), reads=["wvst%d" % s], writes=["wvbf%d" % s])
        for t in range(T // 128):
            bank = 4 + (t % 2)
            ps = g.ps[bank]
            pk = "ps%d" % bank
            for k in range(8):
                p.op("pe", lambda e, ps=ps, s=s, k=k, t=t: e.matmul(ps[:], lhsT=xnT[:, k, t * 128:(t + 1) * 128], rhs=wvbf[s][:, k, :],
                                                                   start=(k == 0), stop=(k == 7)),
                     reads=["wvbf%d" % s, "xnT"], writes=[pk])
            n_ep += 1
            oi = n_ep % 4
            o = ost[oi]
            ok = "ost%d" % oi
            if t % 2 == 0:
                p.op("act", lambda e, o=o, ps=ps: e.activation(out=o[:], in_=ps[:], func=AF.Copy), reads=[pk], writes=[ok])
            else:
                p.op("dve", lambda e, o=o, ps=ps: e.tensor_copy(out=o[:], in_=ps[:]), reads=[pk], writes=[ok])
            dst = g.cv[b] if ctx else g.v[b]
            p.dma("act", dst[t * 128:(t + 1) * 128, vc * 512:(vc + 1) * 512], o[:], reads=[ok], writes=["scr"])
    p.pop()


def phase_gla(g, b, ctx):
    p = g.p
    T = CTX if ctx else L
    NCH = T // 128
    kT_src = g.ckT[b] if ctx else g.kT[b]
    ua_src = g.cuaT[b] if ctx else g.uaT[b]
    v_src = g.cv[b] if ctx else g.v[b]
    p.push()
    aw2f = p.sbuf("aw2f", [32, 2, 512], F32)
    aw2 = p.sbuf("aw2", [32, 2, 512], BF16)
    abT = p.sbuf("abT", [128, 2, 4], F32)
    nabT = p.sbuf("nabT", [128, 2, 4], F32)
    gnw = p.sbuf("gnw", [128, 4, 2], F32)
    mkf = p.sbuf("mkf", [128, 128], F32)
    mkb = p.sbuf("mkb", [128, 128], F32)
    ones_b = p.sbuf("ones_b", [128, 128], BF16)
    m01 = p.sbuf("m01", [128, T], BF16)
    uaf = p.sbuf("uaf", [32, 256], F32)
    uab = p.sbuf("uab", [32, T], BF16)
    A1 = p.sbuf("A1", [128, T], F32)
    A2 = p.sbuf("A2", [128, T], F32)
    E1 = p.sbuf("E1", [128, T], BF16)
    kT = p.sbuf("kT", [128, T], BF16)
    kf = p.sbuf("kf", [128, T], BF16)
    kb = p.sbuf("kb", [128, T], BF16)
    vh = p.sbuf("vh", [128, NCH, 256], BF16)
    Dq = p.sbuf("Dq", [128, NCH], F32)
    Db = p.sbuf("Db", [128, NCH], F32)
    S32 = p.sbuf("S32", [128, 256], F32)
    Stmp = p.sbuf("Stmp", [128, 256], F32)
    ktk_f = p.sbuf("ktk_f", [128, NCH, 128], BF16)
    ktk_b = p.sbuf("ktk_b", [128, NCH, 128], BF16)
    if not ctx:
        qT = p.sbuf("qT", [128, T], BF16)
        qf = p.sbuf("qf", [128, T], BF16)
        qb = p.sbuf("qb", [128, T], BF16)
        sg = p.sbuf("sg", [128, 2, T], BF16)
        Rbf = p.sbuf("Rbf", [128, NCH, 256], BF16)
        Sbf = [p.sbuf("Sbf%d" % i, [128, 256], BF16) for i in range(2)]
        Acm = [p.sbuf("Acm%d" % i, [128, 128], BF16) for i in range(2)]
        Atmp = p.sbuf("Atmp", [128, 128], F32)
        sq2 = [p.sbuf("sq%d" % i, [128, 2, 128], BF16) for i in range(2)]
        Atm2 = p.sbuf("Atm2", [128, 128], F32)
        rs = p.sbuf("rs", [128, 128], F32)
        ytmp = p.sbuf("ytmp", [128, 2, 128], F32)
    p.dma("sp", aw2f[:], g.a_w2e, writes=["aw2f"])
    p.dma("sp", abT[:], g.a_bT, writes=["abT"])
    p.dma("sp", gnw[:], g.gnwT, writes=["gnw"])
    p.dma("sp", mkf[:], g.maskf, writes=["mkf"])
    p.dma("sp", mkb[:], g.maskb, writes=["mkb"])
    p.op("dve", lambda e: e.tensor_copy(out=aw2[:], in_=aw2f[:]), reads=["aw2f"], writes=["aw2"])
    p.op("dve", lambda e: e.tensor_scalar(out=nabT[:], in0=abT[:], scalar1=-1.0, scalar2=None, op0=ALU.mult), reads=["abT"], writes=["nabT"])
    for i in range(T // 256):
        p.dma("sp", uaf[:], ua_src[:, i * 256:(i + 1) * 256], writes=["uaf"])
        p.op("dve", lambda e, i=i: e.tensor_copy(out=uab[:, i * 256:(i + 1) * 256], in_=uaf[:]), reads=["uaf"], writes=["uab"])
    p.op("pool", lambda e: e.memset(ones_b[:], 1.0), writes=["ones_b"])
    p.op("pool", lambda e: e.memset(m01[:], 1.0), writes=["m01"])
    p.op("pool", lambda e: e.memset(m01[:].rearrange("p (c t) -> p c t", t=128)[:, :, 0:1], 0.0), writes=["m01"])
    TG = 512 if T >= 512 else T

    def softplus_scan(h, d, dst_sp, dst_B):
        for tg in range(T // TG):
            ts = slice(tg * TG, (tg + 1) * TG)
            bank = tg % 2
            ps = g.ps[bank]
            pk = "ps%d" % bank
            p.op("pe", lambda e, ps=ps, ts=ts: e.matmul(ps[:, 0:TG], lhsT=aw2[:, d, h * 128:(h + 1) * 128], rhs=uab[:, ts], start=True, stop=True),
                 reads=["aw2", "uab"], writes=[pk])
            p.op("act", lambda e, ps=ps, ts=ts: e.activation(out=dst_sp[:, ts], in_=ps[:, 0:TG], func=AF.Exp, scale=-1.0, bias=nabT[:, d, h:h + 1]),
                 reads=[pk, "nabT"], writes=["A1"])
        p.op("act", lambda e: e.activation(out=dst_sp[:], in_=dst_sp[:], func=AF.Ln, scale=1.0, bias=1.0), reads=["A1"], writes=["A1"])
        p.op("dve", lambda e: e.tensor_tensor_scan(out=dst_B[:], data0=m01[:], data1=dst_sp[:], initial=0.0, op0=ALU.mult, op1=ALU.add),
             reads=["A1", "m01"], writes=["A2"])

    def do_head(h):
        hk = slice(h * 128, (h + 1) * 128)
        p.dma("sp", kT[:], kT_src[hk, :], writes=["kT"])
        p.dma("sp", vh[:], v_src.rearrange("(c p) n -> p c n", p=128)[:, :, h * 256:(h + 1) * 256], writes=["vh"])
        if not ctx:
            p.dma("sp", qT[:], g.qT[b][hk, :], writes=["qT"])
            p.dma("sp", sg[:], g.sg[b][h * 256:(h + 1) * 256, :].rearrange("(a p) t -> p a t", p=128), writes=["sg"])
        softplus_scan(h, 0, A1, A2)
        A2c = A2[:].rearrange("p (c t) -> p c t", t=128)
        p.op("act", lambda e: e.activation(out=Dq[:], in_=A2c[:, :, 127], func=AF.Exp, scale=-1.0 / 16), reads=["A2"], writes=["Dq"])
        p.op("act", lambda e: e.activation(out=E1[:], in_=A2[:], func=AF.Exp, scale=1.0 / 16), reads=["A2"], writes=["E1"])
        p.op("dve", lambda e: e.tensor_tensor(out=kf[:], in0=kT[:], in1=E1[:], op=ALU.mult), reads=["kT", "E1"], writes=["kf"])
        if not ctx:
            p.op("act", lambda e: e.activation(out=E1[:], in_=A2[:], func=AF.Exp, scale=-1.0 / 16), reads=["A2"], writes=["E1"])
            p.op("dve", lambda e: e.tensor_tensor(out=qf[:], in0=qT[:], in1=E1[:], op=ALU.mult), reads=["qT", "E1"], writes=["qf"])
        softplus_scan(h, 1, A1, A2)
        p.op("dve", lambda e: e.tensor_tensor(out=A2c, in0=A2c[:, :, 127:128].to_broadcast([128, NCH, 128]), in1=A2c, op=ALU.subtract),
             reads=["A2"], writes=["A2"])
        if not ctx:
            p.op("act", lambda e: e.activation(out=E1[:], in_=A2[:], func=AF.Exp, scale=-1.0 / 16), reads=["A2"], writes=["E1"])
            p.op("dve", lambda e: e.tensor_tensor(out=qb[:], in0=qT[:], in1=E1[:], op=ALU.mult), reads=["qT", "E1"], writes=["qb"])
        p.op("dve", lambda e: e.tensor_tensor(out=A1[:], in0=A1[:], in1=A2[:], op=ALU.add), reads=["A1", "A2"], writes=["A1"])
        A1c = A1[:].rearrange("p (c t) -> p c t", t=128)
        p.op("act", lambda e: e.activation(out=Db[:], in_=A1c[:, :, 0], func=AF.Exp, scale=-1.0 / 16), reads=["A1"], writes=["Db"])
        p.op("act", lambda e: e.activation(out=E1[:], in_=A1[:], func=AF.Exp, scale=1.0 / 16), reads=["A1"], writes=["E1"])
        p.op("dve", lambda e: e.tensor_tensor(out=kb[:], in0=kT[:], in1=E1[:], op=ALU.mult), reads=["kT", "E1"], writes=["kb"])

        def make_ktok(ktil, dst, dkey, skey):
            for c4 in range(NCH // 4 if NCH >= 4 else 1):
                n4 = min(4, NCH)
                bank = 2 + c4 % 2
                pv = g.ps[bank][:].bitcast(BF16)
                for j in range(n4):
                    c = c4 * 4 + j
                    p.op("pe", lambda e, c=c, j=j, pv=pv: e.transpose(pv[:, j * 128:(j + 1) * 128], ktil[:, c * 128:(c + 1) * 128], g.ident_b[:]),
                         reads=[skey, "ident_b"], writes=["ps%d" % bank])
                dv = dst[:, c4 * 4:c4 * 4 + n4, :].rearrange("p a b -> p (a b)")
                if c4 % 2 == 0:
                    p.op("act", lambda e, dv=dv, pv=pv, n4=n4: e.activation(out=dv, in_=pv[:, 0:n4 * 128], func=AF.Copy), reads=["ps%d" % bank], writes=[dkey])
                else:
                    p.op("dve", lambda e, dv=dv, pv=pv, n4=n4: e.tensor_copy(out=dv, in_=pv[:, 0:n4 * 128]), reads=["ps%d" % bank], writes=[dkey])

        make_ktok(kb, ktk_b, "ktk_b", "kb")
        make_ktok(kf, ktk_f, "ktk_f", "kf")

        def state_step(ktk, kkey, c, Dv, store_bf=None, n=[0]):
            n[0] += 1
            bank = 2 + n[0] % 2
            pd = g.ps[bank]
            pk = "ps%d" % bank
            p.op("pe", lambda e: e.matmul(pd[:, 0:256], lhsT=ktk[:, c, :], rhs=vh[:, c, :], start=True, stop=True), reads=[kkey, "vh"], writes=[pk])
            p.op("dve", lambda e: e.tensor_tensor(out=Stmp[:], in0=pd[:, 0:256], in1=S32[:], op=ALU.add), reads=[pk, "S32"], writes=["Stmp"])
            p.op("act", lambda e: e.activation(out=S32[:], in_=Stmp[:], func=AF.Copy, scale=Dv[:, c:c + 1]), reads=["Stmp", "Dq", "Db"], writes=["S32"])
            if store_bf is not None:
                dst, key = store_bf
                p.op("act", lambda e: e.activation(out=dst, in_=Stmp[:], func=AF.Copy, scale=Dv[:, c:c + 1]),
                     reads=["Stmp", "Dq", "Db"], writes=[key])

        if ctx:
            p.op("dve", lambda e: e.memset(S32[:], 0.0), writes=["S32"])
        else:
            p.op("dve", lambda e: e.tensor_copy(out=S32[:], in_=g.s0[:, b, 1, h, :]), reads=["s0"], writes=["S32"])
            p.op("act", lambda e: e.activation(out=Rbf[:, NCH - 1, :], in_=g.s0[:, b, 1, h, :], func=AF.Copy), reads=["s0"], writes=["Rbf"])
        for c in range(NCH - 1, -1, -1):
            if ctx or c == 0:
                state_step(ktk_b, "ktk_b", c, Db)
            else:
                state_step(ktk_b, "ktk_b", c, Db, store_bf=(Rbf[:, c - 1, :], "Rbf"))
        if ctx:
            p.op("dve", lambda e: e.tensor_copy(out=g.s0[:, b, 1, h, :], in_=S32[:]), reads=["S32"], writes=["s0"])
        if ctx:
            p.op("dve", lambda e: e.memset(S32[:], 0.0), writes=["S32"])
            for c in range(NCH):
                state_step(ktk_f, "ktk_f", c, Dq)
            p.op("dve", lambda e: e.tensor_copy(out=g.s0[:, b, 0, h, :], in_=S32[:]), reads=["S32"], writes=["s0"])
            return
        p.op("dve", lambda e: e.tensor_copy(out=S32[:], in_=g.s0[:, b, 0, h, :]), reads=["s0"], writes=["S32"])
        p.op("act", lambda e: e.activation(out=Sbf[0][:], in_=g.s0[:, b, 0, h, :], func=AF.Copy), reads=["s0"], writes=["Sbf0"])

        def stA(c):
            ts = slice(c * 128, (c + 1) * 128)
            Ac, Ak = Acm[c % 2], "Acm%d" % (c % 2)
            p.op("pe", lambda e: e.matmul(g.ps[4][:, 0:128], lhsT=kf[:, ts], rhs=qf[:, ts], start=True, stop=True), reads=["kf", "qf"], writes=["ps4"])
            p.op("pe", lambda e: e.matmul(g.ps[5][:, 0:128], lhsT=kb[:, ts], rhs=qb[:, ts], start=True, stop=True), reads=["kb", "qb"], writes=["ps5"])
            p.op("dve", lambda e: e.tensor_tensor(out=Atmp[:], in0=g.ps[4][:, 0:128], in1=mkf[:], op=ALU.mult), reads=["ps4", "mkf"], writes=["Atmp"])
            p.op("dve", lambda e: e.tensor_tensor(out=Atm2[:], in0=g.ps[5][:, 0:128], in1=mkb[:], op=ALU.mult), reads=["ps5", "mkb"], writes=["Atm2"])
            p.op("dve", lambda e: e.tensor_tensor(out=Ac[:], in0=Atmp[:], in1=Atm2[:], op=ALU.add), reads=["Atmp", "Atm2"], writes=[Ak])

        def stO1(c):
            ts = slice(c * 128, (c + 1) * 128)
            Sb, Sk = Sbf[c % 2], "Sbf%d" % (c % 2)
            Ac, Ak = Acm[c % 2], "Acm%d" % (c % 2)
            po, pok = g.ps[6 + c % 2], "ps%d" % (6 + c % 2)
            for half in range(2):
                hs = slice(half * 128, (half + 1) * 128)
                p.op("pe", lambda e, half=half, hs=hs: e.matmul(po[:, half * 128:(half + 1) * 128], lhsT=vh[:, c, hs], rhs=Ac[:], start=True, stop=False),
                     reads=["vh", Ak], writes=[pok])
                p.op("pe", lambda e, half=half, hs=hs: e.matmul(po[:, half * 128:(half + 1) * 128], lhsT=Sb[:, hs], rhs=qf[:, ts], start=False, stop=False),
                     reads=[Sk, "qf"], writes=[pok])
                p.op("pe", lambda e, half=half, hs=hs: e.matmul(po[:, half * 128:(half + 1) * 128], lhsT=Rbf[:, c, hs], rhs=qb[:, ts], start=False, stop=True),
                     reads=["Rbf", "qb"], writes=[pok])
            sq_ = sq2[c % 2]
            p.op("act", lambda e: e.activation(out=sq_[:].rearrange("p a t -> p (a t)"), in_=po[:, 0:256], func=AF.Square), reads=[pok], writes=["sq%d" % (c % 2)])

        def stO2(c):
            ts = slice(c * 128, (c + 1) * 128)
            po, pok = g.ps[6 + c % 2], "ps%d" % (6 + c % 2)
            sq_ = sq2[c % 2]
            pss = g.ps[2 + c % 2]
            psk = "ps%d" % (2 + c % 2)
            for half in range(2):
                p.op("pe", lambda e, half=half: e.matmul(pss[:, 256:384], lhsT=ones_b[:], rhs=sq_[:, half, :], start=(half == 0), stop=(half == 1)),
                     reads=["ones_b", "sq%d" % (c % 2)], writes=[psk])
            p.op("act", lambda e: e.activation(out=rs[:], in_=pss[:, 256:384], func=AF.Ln, scale=1.0 / 256, bias=EPS), reads=[psk], writes=["rs"])
            p.op("act", lambda e: e.activation(out=rs[:], in_=rs[:], func=AF.Exp, scale=-0.5), reads=["rs"], writes=["rs"])
            for half in range(2):
                p.op("dve", lambda e, half=half: e.tensor_tensor(out=ytmp[:, half, :], in0=po[:, half * 128:(half + 1) * 128], in1=rs[:], op=ALU.mult),
                     reads=[pok, "rs"], writes=["ytmp"])
                p.op("dve", lambda e, half=half: e.scalar_tensor_tensor(out=sg[:, half, ts], in0=ytmp[:, half, :], scalar=gnw[:, h, half:half + 1],
                                                                      in1=sg[:, half, ts], op0=ALU.mult, op1=ALU.mult),
                     reads=["ytmp", "gnw", "sg"], writes=["sg"])

        def stS(c):
            Sn, Snk = Sbf[(c + 1) % 2], "Sbf%d" % ((c + 1) % 2)
            state_step(ktk_f, "ktk_f", c, Dq, store_bf=(Sn[:], Snk))

        stA(0)
        for c in range(NCH):
            stO1(c)
            if c + 1 < NCH:
                stA(c + 1)
            if c >= 1:
                stO2(c - 1)
            if c + 1 < NCH:
                stS(c)
        stO2(NCH - 1)
        p.dma("act", g.yglaT[b][h * 256:(h + 1) * 256, :].rearrange("(a p) t -> p a t", p=128), sg[:], reads=["sg"], writes=["scr"])

    for h in range(4):
        do_head(h)
    p.pop()


def phase_filter(g):
    p = g.p
    p.push()
    PI = math.pi
    zx = p.sbuf("zx", [33, 4096], F32)
    h1 = p.sbuf("h1", [64, 4096], F32)
    mt = p.sbuf("mt", [64, 4096], F32)
    h2b = p.sbuf("h2b", [64, 8192], BF16)
    w1 = p.sbuf("w1", [33, 64], F32)
    w2 = p.sbuf("w2", [64, 64], F32)
    w3s = p.sbuf("w3s", [64, 512], F32)
    w3b = p.sbuf("w3b", [64, 4096], BF16)
    bf = p.sbuf("bf", [64, 4], F32)
    fb = p.sbuf("fb", [64, 2], F32)
    dl = p.sbuf("dl", [128, 8], F32)
    ndl = p.sbuf("ndl", [128, 8], F32)
    tx = p.sbuf("tx", [128, 8192], F32)
    dec = p.sbuf("dec", [128, 4096], F32)
    kern = p.sbuf("kern", [128, 8192], F32)
    kbf = p.sbuf("kbf", [128, 8192], BF16)
    st = p.sbuf("st", [128, 4], F32)
    p.dma("sp", w1[:], g.hy_w1, writes=["w1"])
    p.dma("sp", w2[:], g.hy_w2, writes=["w2"])
    p.dma("sp", bf[:], g.hy_bf, writes=["bf"])
    p.dma("sp", dl[:], g.deltaT, writes=["dl"])
    p.dma("sp", tx[:], g.text[0:1, :].partition_broadcast(128), writes=["tx"])
    for i in range(8):
        p.dma("sp", w3s[:], g.hy_w3[:, i * 512:(i + 1) * 512], writes=["w3s"])
        p.op("dve", lambda e, i=i: e.tensor_copy(out=w3b[:, i * 512:(i + 1) * 512], in_=w3s[:]), reads=["w3s"], writes=["w3b"])
    p.op("dve", lambda e: e.tensor_tensor(out=fb[:], in0=bf[:, 0:2], in1=bf[:, 2:4], op=ALU.mult), reads=["bf"], writes=["fb"])
    p.op("dve", lambda e: e.tensor_scalar(out=ndl[:], in0=dl[:], scalar1=-1.0, scalar2=None, op0=ALU.mult), reads=["dl"], writes=["ndl"])

    def sin_layer(src, K, wt, li, dst_f32=None, dst_bf=None):
        for ch in range(8):
            cs = slice(ch * 512, (ch + 1) * 512)
            ps = g.ps[ch % 2]
            pk = "ps%d" % (ch % 2)
            p.op("pe", lambda e, ps=ps, cs=cs: e.matmul(ps[0:64, :], lhsT=wt[0:K, :], rhs=src[0:K, cs], start=True, stop=True),
                 reads=["w1", "w2", "zx", "h1"], writes=[pk])
            p.op("act", lambda e, ps=ps, cs=cs: e.activation(out=mt[:, cs], in_=ps[0:64, :], func=AF.Identity, scale=bf[:, 2 + li:3 + li], bias=fb[:, li:li + 1]),
                 reads=[pk, "bf", "fb"], writes=["mt"])
        tmp = kern[0:64, 0:4096]
        for rep in range(2):
            p.op("dve", lambda e: e.tensor_scalar(out=tmp, in0=mt[:], scalar1=PI, scalar2=-2 * PI, op0=ALU.is_gt, op1=ALU.mult), reads=["mt"], writes=["kern"])
            p.op("dve", lambda e: e.tensor_tensor(out=mt[:], in0=mt[:], in1=tmp, op=ALU.add), reads=["mt", "kern"], writes=["mt"])
            p.op("dve", lambda e: e.tensor_scalar(out=tmp, in0=mt[:], scalar1=-PI, scalar2=2 * PI, op0=ALU.is_lt, op1=ALU.mult), reads=["mt"], writes=["kern"])
            p.op("dve", lambda e: e.tensor_tensor(out=mt[:], in0=mt[:], in1=tmp, op=ALU.add), reads=["mt", "kern"], writes=["mt"])
        p.op("dve", lambda e: e.tensor_scalar(out=mt[:], in0=mt[:], scalar1=PI, scalar2=-PI, op0=ALU.min, op1=ALU.max), reads=["mt"], writes=["mt"])
        if dst_f32 is not None:
            p.op("act", lambda e: e.activation(out=dst_f32, in_=mt[:], func=AF.Sin), reads=["mt"], writes=["h1"])
        else:
            p.op("act", lambda e: e.activation(out=dst_bf, in_=mt[:], func=AF.Sin), reads=["mt"], writes=["h2b"])

    for nh in range(2):
        p.dma("sp", zx[:], g.zext[:, nh * 4096:(nh + 1) * 4096], writes=["zx"])
        sin_layer(zx, 33, w1, 0, dst_f32=h1[:])
        sin_layer(h1, 64, w2, 1, dst_bf=h2b[:, nh * 4096:(nh + 1) * 4096])

    def one(o, cc):
        for nh in range(2):
            od = 2 * o + nh
            p.op("act", lambda e, nh=nh: e.activation(out=dec[:], in_=tx[:, nh * 4096:(nh + 1) * 4096], func=AF.Exp, scale=ndl[:, cc:cc + 1]),
                 reads=["tx", "ndl"], writes=["dec"])
            for ch in range(8):
                ps = g.ps[2 + ch % 4]
                pk = "ps%d" % (2 + ch % 4)
                cs = slice(nh * 4096 + ch * 512, nh * 4096 + (ch + 1) * 512)
                p.op("pe", lambda e, ps=ps, cs=cs, od=od: e.matmul(ps[:], lhsT=w3b[:, od * 1024 + cc * 128:od * 1024 + (cc + 1) * 128], rhs=h2b[:, cs], start=True, stop=True),
                     reads=["w3b", "h2b"], writes=[pk])
                p.op("dve", lambda e, ps=ps, cs=cs, ch=ch: e.tensor_tensor(out=kern[:, cs], in0=ps[:], in1=dec[:, ch * 512:(ch + 1) * 512], op=ALU.mult),
                     reads=[pk, "dec"], writes=["kern"])
            p.op("act", lambda e, nh=nh: e.activation(out=dec[:], in_=kern[:, nh * 4096:(nh + 1) * 4096], func=AF.Square, accum_out=st[:, nh:nh + 1]),
                 reads=["kern"], writes=["dec", "st"])
        p.op("dve", lambda e: e.tensor_tensor(out=st[:, 2:3], in0=st[:, 0:1], in1=st[:, 1:2], op=ALU.add), reads=["st"], writes=["st"])
        p.op("act", lambda e: e.activation(out=st[:, 3:4], in_=st[:, 2:3], func=AF.Ln), reads=["st"], writes=["st"])
        p.op("act", lambda e: e.activation(out=st[:, 3:4], in_=st[:, 3:4], func=AF.Exp, scale=-0.5), reads=["st"], writes=["st"])
        p.op("dve", lambda e: e.tensor_scalar(out=kbf[:], in0=kern[:], scalar1=st[:, 3:4], scalar2=None, op0=ALU.mult), reads=["kern", "st"], writes=["kbf"])
        p.dma("act", g.kernT[o][cc * 128:(cc + 1) * 128, :], kbf[:], reads=["kbf"], writes=["scr"])

    p.dma("sp", g.dbg_h2, h2b[:], reads=["h2b"], writes=["dbgh2"])
    p.dma("sp", g.dbg_h1, h1[:], reads=["h1"], writes=["dbgh1"])
    p.dma("sp", g.dbg_mt, mt[:], reads=["mt"], writes=["dbgmt"])
    for o in range(2):
        for cc in range(8):
            one(o, cc)
            if o == 0 and cc == 0:
                p.dma("sp", g.dbg_kern, kern[:], reads=["kern"], writes=["dbgk"])
                p.dma("sp", g.dbg_st, st[:], reads=["st"], writes=["dbgst"])
    p.pop()


CG = 64


def phase_hyena(g, dbg):
    p = g.p
    p.push()
    stg = p.sbuf("stg", [128, 8, 2, 128], F32)
    Gt = p.sbuf("Gt", [128, 64, 2, 128], BF16)
    Gp = p.sbuf("Gp", [128, 128, 32], BF16)
    F64t = p.sbuf("F64t", [64, 256], BF16)
    C3 = p.sbuf("C3", [128, 3, 128], BF16)
    hbT = p.sbuf("hbT", [64, 16, 2], F32)
    Xs = p.sbuf("Xs", [64, CG, 128], BF16)
    Asb = p.sbuf("Asb", [128, 64, 4, CG], BF16)
    Ssb = p.sbuf("Ssb", [128, 64, 2, CG], BF16)
    V = p.sbuf("V", [128, 2, 64, CG], BF16)
    Ub = [p.sbuf("Ub%d" % i, [128, 4, 2, CG], BF16) for i in range(2)]
    tv = [p.sbuf("tv%d" % i, [128, 2, 4, 2, CG], BF16) for i in range(2)]
    Psb = p.sbuf("Psb", [128, CG, 128], BF16)
    zin = p.sbuf("zin", [CG, L], BF16)
    prt = p.sbuf("prt", [CG, L], BF16)
    zout = p.sbuf("zout", [CG, L], BF16)
    ty = [p.sbuf("ty%d" % i, [CG, 16, 32], F32) for i in range(2)]
    p.dma("sp", hbT[:], g.hy_biasT, writes=["hbT"])
    for i in range(8):
        p.dma("sp", stg[:], g.tG[:, i * 8:(i + 1) * 8, :, :], writes=["stg"])
        p.op("dve", lambda e, i=i: e.tensor_copy(out=Gt[:, i * 8:(i + 1) * 8, :, :], in_=stg[:]), reads=["stg"], writes=["Gt"])
    stg2 = stg[:].rearrange("p a b c -> p (a b c)")
    for i in range(2):
        p.dma("sp", stg2[:, 0:2048], g.tGp[:, i * 64:(i + 1) * 64, :].rearrange("p a b -> p (a b)"), writes=["stg"])
        p.op("dve", lambda e, i=i: e.tensor_copy(out=Gp[:, i * 64:(i + 1) * 64, :].rearrange("p a b -> p (a b)"), in_=stg2[:, 0:2048]), reads=["stg"], writes=["Gp"])
    p.dma("sp", stg2[0:64, 0:256], g.tF64, writes=["stg"])
    p.op("dve", lambda e: e.tensor_copy(out=F64t[:], in_=stg2[0:64, 0:256]), reads=["stg"], writes=["F64t"])
    p.dma("sp", stg2[:, 0:384], g.tC3.rearrange("p a b -> p (a b)"), writes=["stg"])
    p.op("dve", lambda e: e.tensor_copy(out=C3[:].rearrange("p a b -> p (a b)"), in_=stg2[:, 0:384]), reads=["stg"], writes=["C3"])

    cnt = [0]

    def fwd_stage1(src_rows, K):
        if src_rows is not None:
            p.dma("sp", Xs[0:K, :, :], src_rows.rearrange("c (n1 n2) -> n1 c n2", n2=128), writes=["Xs"])
        for c2 in range(CG // 2):
            bank = c2 % 2
            ps = g.ps[bank]
            pk = "ps%d" % bank
            for j in range(2):
                c = c2 * 2 + j
                p.op("pe", lambda e, ps=ps, c=c, j=j: e.matmul(ps[:, j * 256:(j + 1) * 256], lhsT=Xs[0:K, c, :], rhs=F64t[0:K, :], start=True, stop=True),
                     reads=["Xs", "F64t"], writes=[pk])
            src = ps[:, 0:512].rearrange("p (c m k) -> p k m c", c=2, m=4)
            dst = Asb[:, :, :, c2 * 2:c2 * 2 + 2]
            if c2 % 2 == 0:
                p.op("act", lambda e, src=src, dst=dst: e.activation(out=dst, in_=src, func=AF.Copy), reads=[pk], writes=["Asb"])
            else:
                p.op("dve", lambda e, src=src, dst=dst: e.tensor_copy(out=dst, in_=src), reads=[pk], writes=["Asb"])

    def fwd_stage2(k1, bank, q):
        ps = g.ps[bank]
        pk = "ps%d" % bank
        u = ps[:, q * 128:q * 128 + 2 * CG]
        p.op("pe", lambda e: e.matmul(u, lhsT=Gt[:, k1, 0, :], rhs=Asb[:, k1, 0:2, :].rearrange("p m c -> p (m c)"), start=True, stop=False), reads=["Gt", "Asb"], writes=[pk])
        p.op("pe", lambda e: e.matmul(u, lhsT=Gt[:, k1, 1, :], rhs=Asb[:, k1, 2:4, :].rearrange("p m c -> p (m c)"), start=False, stop=True), reads=["Gt", "Asb"], writes=[pk])

    def fwd_stage2_block(kb):
        cnt[0] += 1
        bank = 2 + cnt[0] % 2
        for q in range(4):
            fwd_stage2(kb * 4 + q, bank, q)
        return g.ps[bank][:].rearrange("p (k m c) -> p k m c", k=4, m=2), "ps%d" % bank

    for o in range(2):
        for cgi in range(16):
            fwd_stage1(g.kernT[o][cgi * CG:(cgi + 1) * CG, :], 64)
            for kb in range(16):
                u, pk = fwd_stage2_block(kb)
                if kb % 2 == 0:
                    p.op("act", lambda e, u=u, kb=kb: e.activation(out=Ssb[:, kb * 4:(kb + 1) * 4, :, :], in_=u, func=AF.Copy), reads=[pk], writes=["Ssb"])
                else:
                    p.op("dve", lambda e, u=u, kb=kb: e.tensor_copy(out=Ssb[:, kb * 4:(kb + 1) * 4, :, :], in_=u), reads=[pk], writes=["Ssb"])
            p.dma("act", g.sspec[o][cgi], Ssb[:].rearrange("p a b c -> p (a b c)"), reads=["Ssb"], writes=["sspec"])
    if "stop6" in dbg:
        p.pop()
        return

    zin2 = [zin, p.sbuf("zin_b", [CG, L], BF16)]
    prt2 = [prt, p.sbuf("prt_b", [CG, L], BF16)]
    units = [(b, o, cgi) for b in range(NB) for o in range(2) for cgi in range(16)]
    if "stop7" in dbg:
        units = units[:16]

    def srcdst(b, o):
        return (g.uhy[b][0] if o == 0 else g.z1T[b]), (g.z1T[b] if o == 0 else g.yhyT[b])

    def stA(i):
        b, o, cgi = units[i]
        rows = slice(cgi * CG, (cgi + 1) * CG)
        zsrc, _ = srcdst(b, o)
        zk = [("z", b, o - 1, cgi)] if o == 1 else []
        p.dma("sp", Xs[0:32, :, :], zsrc[rows, :].rearrange("c (n1 n2) -> n1 c n2", n2=128), reads=zk, writes=["Xs"])
        p.dma("sp", Ssb[:].rearrange("p a b c -> p (a b c)"), g.sspec[o][cgi], writes=["Ssb"])
        p.dma("sp", zin2[i % 2][:], zsrc[rows, :], reads=zk, writes=["zin%d" % (i % 2)])
        p.dma("sp", prt2[i % 2][:], g.uhy[b][o + 1][rows, :], writes=["prt%d" % (i % 2)])
        fwd_stage1(None, 32)

    def stB(i):
        for kb in range(16):
            u, pk = fwd_stage2_block(kb)
            ub, uk = Ub[kb % 2], "Ub%d" % (kb % 2)
            t, tk = tv[kb % 2], "tv%d" % (kb % 2)
            ks = slice(kb * 4, (kb + 1) * 4)
            p.op("act", lambda e, u=u, ub=ub: e.activation(out=ub[:], in_=u, func=AF.Copy), reads=[pk], writes=[uk])
            p.op("dve", lambda e, ub=ub, t=t, ks=ks: e.tensor_tensor(out=t[:, 0], in0=ub[:], in1=Ssb[:, ks, 0:1, :].to_broadcast([128, 4, 2, CG]), op=ALU.mult),
                 reads=[uk, "Ssb"], writes=[tk])
            p.op("dve", lambda e, ub=ub, t=t, ks=ks: e.tensor_tensor(out=t[:, 1], in0=ub[:], in1=Ssb[:, ks, 1:2, :].to_broadcast([128, 4, 2, CG]), op=ALU.mult),
                 reads=[uk, "Ssb"], writes=[tk])
            p.op("pool", lambda e, t=t, ks=ks: e.tensor_tensor(out=V[:, 0, ks, :], in0=t[:, 0, :, 0, :], in1=t[:, 1, :, 1, :], op=ALU.subtract), reads=[tk], writes=["V"])
            p.op("dve", lambda e, t=t, ks=ks: e.tensor_tensor(out=V[:, 1, ks, :], in0=t[:, 1, :, 0, :], in1=t[:, 0, :, 1, :], op=ALU.add), reads=[tk], writes=["V"])

    def stC(i):
        for c4 in range(CG // 4):
            bank = 4 + c4 % 2
            ps = g.ps[bank]
            pk = "ps%d" % bank
            for j in range(4):
                c = c4 * 4 + j
                cs = slice(j * 128, (j + 1) * 128)
                p.op("pe", lambda e, ps=ps, c=c, cs=cs: e.matmul(ps[0:64, cs], lhsT=V[:, 0, :, c], rhs=C3[:, 0, :], start=True, stop=False), reads=["V", "C3"], writes=[pk])
                p.op("pe", lambda e, ps=ps, c=c, cs=cs: e.matmul(ps[0:64, cs], lhsT=V[:, 1, :, c], rhs=C3[:, 2, :], start=False, stop=True), reads=["V", "C3"], writes=[pk])
                p.op("pe", lambda e, ps=ps, c=c, cs=cs: e.matmul(ps[64:128, cs], lhsT=V[:, 0, :, c], rhs=C3[:, 1, :], start=True, stop=False), reads=["V", "C3"], writes=[pk])
                p.op("pe", lambda e, ps=ps, c=c, cs=cs: e.matmul(ps[64:128, cs], lhsT=V[:, 1, :, c], rhs=C3[:, 0, :], start=False, stop=True), reads=["V", "C3"], writes=[pk])
            dst = Psb[:, c4 * 4:(c4 + 1) * 4, :].rearrange("p a b -> p (a b)")
            if c4 % 2 == 0:
                p.op("act", lambda e, ps=ps, dst=dst: e.activation(out=dst, in_=ps[:], func=AF.Copy), reads=[pk], writes=["Psb"])
            else:
                p.op("dve", lambda e, ps=ps, dst=dst: e.tensor_copy(out=dst, in_=ps[:]), reads=[pk], writes=["Psb"])

    def stD(i):
        b, o, cgi = units[i]
        rows = slice(cgi * CG, (cgi + 1) * CG)
        _, zdst = srcdst(b, o)
        zi, pr_ = zin2[i % 2], prt2[i % 2]
        zin_v = zi[:].rearrange("c (tb ta) -> c ta tb", ta=128)
        prt_v = pr_[:].rearrange("c (tb ta) -> c ta tb", ta=128)
        zout_v = zout[:].rearrange("c (tb ta) -> c ta tb", ta=128)
        for r in range(8):
            bank = 6 + r % 2
            ps = g.ps[bank]
            pk = "ps%d" % bank
            for j in range(16):
                ta = r * 16 + j
                p.op("pe", lambda e, ps=ps, ta=ta, j=j: e.matmul(ps[0:CG, j * 32:(j + 1) * 32], lhsT=Psb[:, :, ta], rhs=Gp[:, ta, :], start=True, stop=True),
                     reads=["Psb", "Gp"], writes=[pk])
            t = ty[r % 2]
            tk = "ty%d" % (r % 2)
            tas = slice(r * 16, (r + 1) * 16)
            p.op("dve", lambda e, ps=ps, t=t, tas=tas: e.scalar_tensor_tensor(out=t[:], in0=zin_v[:, tas, :], scalar=hbT[:, cgi, o:o + 1],
                                                                            in1=ps[0:CG, :].rearrange("c (a b) -> c a b", b=32), op0=ALU.mult, op1=ALU.add),
                 reads=[pk, "zin%d" % (i % 2), "hbT"], writes=[tk])
            p.op("pool", lambda e, t=t, tas=tas: e.tensor_tensor(out=zout_v[:, tas, :], in0=t[:], in1=prt_v[:, tas, :], op=ALU.mult),
                 reads=[tk, "prt%d" % (i % 2)], writes=["zout"])
        p.dma("act", zdst[rows, :], zout[:], reads=["zout"], writes=[("z", b, o, cgi)])

    stA(0)
    for i in range(len(units)):
        stB(i)
        if i + 1 < len(units):
            stA(i + 1)
        stC(i)
        stD(i)
    p.pop()


def phase_merge(g, b, dbg=()):
    p = g.p
    p.push()
    wst = p.sbuf("wst", [128, 8, 512], F32)
    phb = p.sbuf("phb", [128, 8, D], BF16)
    pgb = p.sbuf("pgb", [128, 8, D], BF16)
    wob = p.sbuf("wob", [128, 8, D], BF16)
    g1b = p.sbuf("g1b", [128, D], F32)
    rw = p.sbuf("rw", [128, 8, 64], F32)
    rb = p.sbuf("rb", [128, 64], F32)
    yh = p.sbuf("yh", [128, 8, 512], BF16)
    yg = p.sbuf("yg", [128, 8, 512], BF16)
    gt = p.sbuf("gt", [128, 16, 512], BF16)
    mg = p.sbuf("mg", [128, 8, 512], BF16)
    t1 = p.sbuf("t1", [128, 512], F32)
    t2 = p.sbuf("t2", [128, 512], F32)
    xt = [p.sbuf("xt%d" % i, [128, D], F32) for i in range(2)]
    x1 = [p.sbuf("x1_%d" % i, [128, D], F32) for i in range(2)]
    xs = p.sbuf("xs", [128, D], F32)
    junk = p.sbuf("junk", [128, D], F32)
    stat = p.sbuf("stat", [128, 8], F32)
    xnf = p.sbuf("xnf", [128, 8, 128], F32)
    xnb = [p.sbuf("xnb%d" % i, [128, 8, 128], BF16) for i in range(2)]
    sc = p.sbuf("sc", [128, 64], F32)
    sl = p.sbuf("sl", [128, 64], F32)
    sl2 = p.sbuf("sl2", [128, 64], F32)
    eq = p.sbuf("eq", [128, 64], F32)
    gs = p.sbuf("gs", [128, 8], F32)
    m1 = p.sbuf("m1", [128, 8], F32)
    m2 = p.sbuf("m2", [128, 8], F32)
    gm = p.sbuf("gm", [128, 8], F32)
    mx = p.sbuf("mx", [128, 8], F32)
    cmb = p.sbuf("cmb", [128, 64], F32)
    cmT = [p.sbuf("cmT%d" % i, [64, 128], BF16) for i in range(2)]
    p.dma("sp", g1b[:], g.gbc_d[:, b * D:(b + 1) * D], writes=["g1b"])
    p.dma("sp", rw[:], g.router_w.rearrange("(k p) n -> p k n", p=128), writes=["rw"])
    p.dma("sp", rb[:], g.router_b[0:1, :].partition_broadcast(128), writes=["rb"])
    for wi, (src, dst, key) in enumerate([(g.proj_hy, phb, "phb"), (g.proj_gla, pgb, "pgb"), (g.w_out, wob, "wob")]):
        sv = src.rearrange("(k p) n -> p k n", p=128)
        for hf in range(2):
            cs = slice(hf * 512, (hf + 1) * 512)
            p.dma("sp", wst[:], sv[:, :, cs], writes=["wst"])
            if wi < 2:
                p.op("pool", lambda e, dst=dst, cs=cs: e.tensor_copy(out=dst[:, :, cs], in_=wst[:]), reads=["wst"], writes=[key])
            else:
                p.op("dve", lambda e, dst=dst, cs=cs: e.tensor_tensor(out=dst[:, :, cs], in0=wst[:], in1=g1b[:, cs].unsqueeze(1).to_broadcast([128, 8, 512]), op=ALU.mult),
                     reads=["wst", "g1b"], writes=[key])
    sc2 = [sc, p.sbuf("sc_b", [128, 64], F32)]
    NTILE = L // 128

    def stG(tg):
        ts = slice(tg * 512, (tg + 1) * 512)
        p.dma("sp", yh[:], g.yhyT[b].rearrange("(k p) t -> p k t", p=128)[:, :, ts], writes=["yh"])
        p.dma("sp", yg[:], g.yglaT[b].rearrange("(k p) t -> p k t", p=128)[:, :, ts], writes=["yg"])
        p.dma("sp", gt[:], g.gate[b].rearrange("(k p) t -> p k t", p=128)[:, :, ts], writes=["gt"])
        for dc in range(8):
            ds_ = slice(dc * 128, (dc + 1) * 128)
            pa, pb = g.ps[dc % 2], g.ps[2]
            ka = "ps%d" % (dc % 2)
            for k in range(8):
                p.op("pe", lambda e, pa=pa, k=k, ds_=ds_: e.matmul(pa[:], lhsT=phb[:, k, ds_], rhs=yh[:, k, :], start=(k == 0), stop=(k == 7)), reads=["phb", "yh"], writes=[ka])
            for k in range(8):
                p.op("pe", lambda e, pb=pb, k=k, ds_=ds_: e.matmul(pb[:], lhsT=pgb[:, k, ds_], rhs=yg[:, k, :], start=(k == 0), stop=(k == 7)), reads=["pgb", "yg"], writes=["ps2"])
            p.op("dve", lambda e, pa=pa, dc=dc: e.tensor_tensor(out=t1[:], in0=pa[:], in1=gt[:, dc, :], op=ALU.mult), reads=[ka, "gt"], writes=["t1"])
            p.op("dve", lambda e, pb=pb, dc=dc: e.tensor_tensor(out=t2[:], in0=pb[:], in1=gt[:, 8 + dc, :], op=ALU.mult), reads=["ps2", "gt"], writes=["t2"])
            p.op("pool", lambda e, dc=dc: e.tensor_tensor(out=mg[:, dc, :], in0=t1[:], in1=t2[:], op=ALU.add), reads=["t1", "t2"], writes=["mg"])

    def stY(t):
        i = t % 2
        tt = t % 4
        p.dma("sp", xt[i][:], g.x[b][t * 128:(t + 1) * 128, :], writes=["xt%d" % i])
        for hf in range(2):
            for k in range(8):
                p.op("pe", lambda e, hf=hf, k=k: e.matmul(g.ps[4 + hf][:], lhsT=mg[:, k, tt * 128:(tt + 1) * 128], rhs=wob[:, k, hf * 512:(hf + 1) * 512],
                                                          start=(k == 0), stop=(k == 7)), reads=["mg", "wob"], writes=["ps%d" % (4 + hf)])

    def stT1(t):
        i = t % 2
        xt_, x1_ = xt[i], x1[i]
        tsl = slice(t * 128, (t + 1) * 128)
        for hf in range(2):
            cs = slice(hf * 512, (hf + 1) * 512)
            p.op("dve", lambda e, hf=hf, cs=cs: e.tensor_tensor(out=x1_[:, cs], in0=g.ps[4 + hf][:], in1=xt_[:, cs], op=ALU.add),
                 reads=["ps%d" % (4 + hf), "xt%d" % i], writes=["x1_%d" % i])
        p.dma("act", g.x1d[b][tsl, :], x1_[:], reads=["x1_%d" % i], writes=["scr"])
        p.op("act", lambda e: e.activation(out=junk[:], in_=x1_[:], func=AF.Square, accum_out=stat[:, 0:1]), reads=["x1_%d" % i], writes=["junk", "stat"])
        p.op("act", lambda e: e.activation(out=stat[:, 1:2], in_=stat[:, 0:1], func=AF.Ln, scale=1.0 / D, bias=EPS), reads=["stat"], writes=["stat"])
        p.op("act", lambda e: e.activation(out=stat[:, 2:3], in_=stat[:, 1:2], func=AF.Exp, scale=-0.5), reads=["stat"], writes=["stat"])
        p.op("dve", lambda e: e.tensor_scalar(out=xs[:], in0=x1_[:], scalar1=stat[:, 2:3], scalar2=None, op0=ALU.mult), reads=["x1_%d" % i, "stat"], writes=["xs"])

    def stT2(t):
        i = t % 2
        xb_ = xnb[i]
        tsl = slice(t * 128, (t + 1) * 128)
        for k2 in range(2):
            pt = g.ps[6 + k2]
            for kk in range(4):
                k = k2 * 4 + kk
                p.op("pe", lambda e, pt=pt, k=k, kk=kk: e.transpose(pt[:, kk * 128:(kk + 1) * 128], xs[:, k * 128:(k + 1) * 128], g.ident_f[:]),
                     reads=["xs", "ident_f"], writes=["ps%d" % (6 + k2)])
            for kk in range(4):
                k = k2 * 4 + kk
                p.op("act", lambda e, pt=pt, k=k, kk=kk: e.activation(out=xnf[:, k, :], in_=pt[:, kk * 128:(kk + 1) * 128], func=AF.Identity,
                                                                    scale=g.scale2T[:, k, b:b + 1], bias=g.modT[:, 24 + k, b:b + 1]),
                     reads=["ps%d" % (6 + k2)], writes=["xnf"])
        p.op("pool", lambda e: e.tensor_copy(out=xb_[:], in_=xnf[:]), reads=["xnf"], writes=["xnb%d" % i])
        p.dma("act", g.xn2T[b].rearrange("(k p) t -> p k t", p=128)[:, :, tsl], xb_[:], reads=["xnb%d" % i], writes=["scr2"])
        pr = g.ps[3]
        for k in range(8):
            p.op("pe", lambda e, k=k: e.matmul(pr[:, 0:64], lhsT=xnf[:, k, :], rhs=rw[:, k, :], start=(k == 0), stop=(k == 7)), reads=["xnf", "rw"], writes=["ps3a"])
        p.op("act", lambda e: e.activation(out=sc2[i][:], in_=pr[:, 0:64], func=AF.Sigmoid), reads=["ps3a"], writes=["sc%d" % i])
        if "dbg_sc" in dbg:
            p.dma("act", g.dbg_sc[tsl, :], sc2[i][:], reads=["sc%d" % i], writes=["dbgsc"])

    def stT3(t):
        i = t % 2
        sc_ = sc2[i]
        sk = "sc%d" % i
        cT_ = cmT[i]
        tsl = slice(t * 128, (t + 1) * 128)
        p.op("dve", lambda e: e.tensor_tensor(out=sl[:], in0=sc_[:], in1=rb[:], op=ALU.add), reads=[sk, "rb"], writes=["sl"])
        sl3 = sl[:].rearrange("p (a b) -> p a b", b=8)
        p.op("dve", lambda e: e.tensor_reduce(out=m1[:], in_=sl3, axis=AX.X, op=ALU.max), reads=["sl"], writes=["m1"])
        p.op("dve", lambda e: e.tensor_tensor(out=eq[:].rearrange("p (a b) -> p a b", b=8), in0=sl3, in1=m1[:].unsqueeze(2).to_broadcast([128, 8, 8]), op=ALU.is_equal),
             reads=["sl", "m1"], writes=["eq"])
        p.op("dve", lambda e: e.scalar_tensor_tensor(out=sl2[:], in0=eq[:], scalar=-1.0e9, in1=sl[:], op0=ALU.mult, op1=ALU.add), reads=["eq", "sl"], writes=["sl2"])
        p.op("dve", lambda e: e.tensor_reduce(out=m2[:], in_=sl2[:].rearrange("p (a b) -> p a b", b=8), axis=AX.X, op=ALU.max), reads=["sl2"], writes=["m2"])
        p.op("dve", lambda e: e.tensor_tensor(out=gs[:], in0=m1[:], in1=m2[:], op=ALU.add), reads=["m1", "m2"], writes=["gs"])
        p.op("dve", lambda e: e.max(out=mx[:], in_=gs[:]), reads=["gs"], writes=["mx"])
        p.op("dve", lambda e: e.tensor_scalar(out=gm[:], in0=gs[:], scalar1=mx[:, 3:4], scalar2=None, op0=ALU.is_ge), reads=["gs", "mx"], writes=["gm"])
        p.op("dve", lambda e: e.scalar_tensor_tensor(out=sl2[:].rearrange("p (a b) -> p a b", b=8), in0=sl3, scalar=10.0,
                                                     in1=gm[:].unsqueeze(2).to_broadcast([128, 8, 8]), op0=ALU.add, op1=ALU.mult),
             reads=["sl", "gm"], writes=["sl2"])
        p.op("dve", lambda e: e.max(out=mx[:], in_=sl2[:]), reads=["sl2"], writes=["mx"])
        p.op("dve", lambda e: e.tensor_scalar(out=eq[:], in0=sl2[:], scalar1=mx[:, 7:8], scalar2=None, op0=ALU.is_ge), reads=["sl2", "mx"], writes=["eq"])
        p.op("dve", lambda e: e.tensor_tensor(out=cmb[:], in0=sc_[:], in1=eq[:], op=ALU.mult), reads=[sk, "eq"], writes=["cmb"])
        p.op("dve", lambda e: e.tensor_reduce(out=stat[:, 4:5], in_=cmb[:], axis=AX.X, op=ALU.add), reads=["cmb"], writes=["stat4"])
        p.op("dve", lambda e: e.reciprocal(out=stat[:, 5:6], in_=stat[:, 4:5]), reads=["stat4"], writes=["stat5"])
        p.op("dve", lambda e: e.tensor_scalar(out=cmb[:], in0=cmb[:], scalar1=stat[:, 5:6], scalar2=2.5, op0=ALU.mult, op1=ALU.mult), reads=["cmb", "stat5"], writes=["cmb"])
        p.op("pe", lambda e: e.transpose(g.ps[3][0:64, 128:256], cmb[:], g.ident_f[:]), reads=["cmb", "ident_f"], writes=["ps3b"])
        p.op("act", lambda e: e.activation(out=cT_[:], in_=g.ps[3][0:64, 128:256], func=AF.Copy), reads=["ps3b"], writes=["cmT%d" % i])
        p.dma("act", g.combT[b][:, tsl], cT_[:], reads=["cmT%d" % i], writes=["scr3"])

    stG(0)
    stY(0)
    stT1(0)
    for t in range(NTILE):
        if t + 1 < NTILE:
            if (t + 1) % 4 == 0:
                stG((t + 1) // 4)
            stY(t + 1)
        stT2(t)
        if t + 1 < NTILE:
            stT1(t + 1)
        stT3(t)
    p.pop()


def phase_moe(g, b, dbg=()):
    p = g.p
    TGM = 2048
    NT = TGM // 128
    SG = 256
    groups = [[2 * i, 2 * i + 1] for i in range(32)] + [[64]]
    if "moe1" in dbg:
        groups = groups[:1] + groups[-1:]
    for tgi in range(L // TGM):
        p.push()
        t0 = tgi * TGM
        xn = p.sbuf("xn", [128, 8, TGM], BF16)
        yacc = p.sbuf("yacc", [128, NT, D], F32)
        cT = p.sbuf("cT", [64, TGM], BF16)
        sel = p.sbuf("sel", [64, 64, 128], BF16)
        p.push()
        wst1 = p.sbuf("wst1", [128, 8, 256], F32)
        wst3 = p.sbuf("wst3", [128, 8, 256], F32)
        wst2 = p.sbuf("wst2", [128, 2, D], F32)
        w1b = [p.sbuf("w1b%d" % i, [128, 8, 256], BF16) for i in range(4)]
        w3b = [p.sbuf("w3b%d" % i, [128, 8, 256], BF16) for i in range(4)]
        w2b = [p.sbuf("w2b%d" % i, [128, 2, D], BF16) for i in range(4)]
        sl = [p.sbuf("sl%d" % i, [128, 2, SG], F32) for i in range(2)]
        aT = [p.sbuf("aT%d" % i, [128, 2, SG], BF16) for i in range(2)]
        p.dma("sp", xn[:], g.xn2T[b].rearrange("(k p) t -> p k t", p=128)[:, :, t0:t0 + TGM], writes=["xn"])
        p.dma("sp", cT[:], g.combT[b][:, t0:t0 + TGM], writes=["cT"])
        for i in range(4):
            self_f = wst1[0:64, :, :].rearrange("p a b -> p (a b)")
            p.dma("sp", self_f, g.sel[:, i * 16:(i + 1) * 16, :].rearrange("p a b -> p (a b)"), writes=["wst1"])
            p.op("dve", lambda e, i=i, self_f=self_f: e.tensor_copy(out=sel[:, i * 16:(i + 1) * 16, :].rearrange("p a b -> p (a b)"), in_=self_f), reads=["wst1"], writes=["sel"])

        def load(gi):
            for j, e_ in enumerate(groups[gi]):
                s = (2 * gi + j) % 4
                if e_ < 64:
                    s1 = g.exp_w1[e_].rearrange("(k p) f -> p k f", p=128)
                    s3 = g.exp_w3[e_].rearrange("(k p) f -> p k f", p=128)
                    s2 = g.exp_w2[e_].rearrange("(k p) d -> p k d", p=128)
                else:
                    s1 = g.sh_w1.rearrange("(k p) f -> p k f", p=128)
                    s3 = g.sh_w3.rearrange("(k p) f -> p k f", p=128)
                    s2 = g.sh_w2.rearrange("(k p) d -> p k d", p=128)
                p.dma("sp", wst1[:], s1, writes=["wst1"])
                p.dma("sp", wst3[:], s3, writes=["wst3"])
                p.dma("sp", wst2[:], s2, writes=["wst2"])
                if gi < 2:
                    p.op("act", lambda e, s=s: e.activation(out=w1b[s][:], in_=wst1[:], func=AF.Copy), reads=["wst1"], writes=["w1b%d" % s])
                    p.op("dve", lambda e, s=s: e.tensor_copy(out=w3b[s][:], in_=wst3[:]), reads=["wst3"], writes=["w3b%d" % s])
                else:
                    p.op("pool", lambda e, s=s: e.tensor_copy(out=w1b[s][:], in_=wst1[:]), reads=["wst1"], writes=["w1b%d" % s])
                    p.op("pool", lambda e, s=s: e.tensor_copy(out=w3b[s][:], in_=wst3[:]), reads=["wst3"], writes=["w3b%d" % s])
                p.op("pool", lambda e, s=s: e.tensor_copy(out=w2b[s][:], in_=wst2[:]), reads=["wst2"], writes=["w2b%d" % s])

        ycnt = [0]
        sl4 = [sl[0], sl[1], p.sbuf("sl2", [128, 2, SG], F32), p.sbuf("sl3", [128, 2, SG], F32)]
        aT4 = [aT[0], aT[1], p.sbuf("aT2", [128, 2, SG], BF16), p.sbuf("aT3", [128, 2, SG], BF16)]

        def stepH(gi, sg_):
            exps = groups[gi]
            par = sg_ % 2
            ts = slice(sg_ * SG, (sg_ + 1) * SG)
            for j, e_ in enumerate(exps):
                s = (2 * gi + j) % 4
                jj = 2 * par + j
                ph1, ph3 = g.ps[2 * j], g.ps[2 * j + 1]
                k1, k3 = "ps%d" % (2 * j), "ps%d" % (2 * j + 1)
                pc = g.ps[4][:, j * SG:(j + 1) * SG]
                for fc in range(2):
                    fs = slice(fc * 128, (fc + 1) * 128)
                    for k in range(8):
                        p.op("pe", lambda e, fc=fc, fs=fs, k=k, s=s, ph1=ph1: e.matmul(ph1[:, fc * SG:(fc + 1) * SG], lhsT=w1b[s][:, k, fs], rhs=xn[:, k, ts],
                                                                                   start=(k == 0), stop=(k == 7)), reads=["w1b%d" % s, "xn"], writes=[k1])
                for fc in range(2):
                    fs = slice(fc * 128, (fc + 1) * 128)
                    for k in range(8):
                        p.op("pe", lambda e, fc=fc, fs=fs, k=k, s=s, ph3=ph3: e.matmul(ph3[:, fc * SG:(fc + 1) * SG], lhsT=w3b[s][:, k, fs], rhs=xn[:, k, ts],
                                                                                   start=(k == 0), stop=(k == 7)), reads=["w3b%d" % s, "xn"], writes=[k3])
                if e_ < 64:
                    p.op("pe", lambda e, e_=e_, pc=pc: e.matmul(pc, lhsT=sel[:, e_, :], rhs=cT[:, ts], start=True, stop=True), reads=["sel", "cT"], writes=["ps4_%d" % j])
                slv = sl4[jj][:].rearrange("p a b -> p (a b)")
                p.op("act", lambda e, slv=slv, ph1=ph1: e.activation(out=slv, in_=ph1[:], func=AF.Silu), reads=[k1], writes=["sl%d" % jj])
                if e_ < 64:
                    p.op("dve", lambda e, slv=slv, ph3=ph3: e.tensor_tensor(out=slv, in0=ph3[:], in1=slv, op=ALU.mult), reads=[k3, "sl%d" % jj], writes=["sl%d" % jj])
                    p.op("dve", lambda e, jj=jj, pc=pc: e.tensor_tensor(out=aT4[jj][:], in0=sl4[jj][:], in1=pc.unsqueeze(1).to_broadcast([128, 2, SG]), op=ALU.mult),
                         reads=["ps4_%d" % j, "sl%d" % jj], writes=["aT%d" % jj])
                else:
                    p.op("dve", lambda e, jj=jj, slv=slv, ph3=ph3: e.tensor_tensor(out=aT4[jj][:].rearrange("p a b -> p (a b)"), in0=ph3[:], in1=slv, op=ALU.mult),
                         reads=[k3, "sl%d" % jj], writes=["aT%d" % jj])

        def stepY(gi, sg_):
            exps = groups[gi]
            par = sg_ % 2
            for tt in range(SG // 128):
                tl = sg_ * (SG // 128) + tt
                for hf in range(2):
                    ycnt[0] += 1
                    bank = 5 + ycnt[0] % 3
                    py = g.ps[bank]
                    nmm = 2 * len(exps)
                    i = 0
                    for j, e_ in enumerate(exps):
                        s = (2 * gi + j) % 4
                        jj = 2 * par + j
                        for fc in range(2):
                            p.op("pe", lambda e, py=py, fc=fc, tt=tt, hf=hf, jj=jj, s=s, i=i, nmm=nmm: e.matmul(
                                py[:], lhsT=aT4[jj][:, fc, tt * 128:(tt + 1) * 128], rhs=w2b[s][:, fc, hf * 512:(hf + 1) * 512],
                                start=(i == 0), stop=(i == nmm - 1)), reads=["aT%d" % jj, "w2b%d" % s], writes=["ps%d" % bank])
                            i += 1
                    dst = yacc[:, tl, hf * 512:(hf + 1) * 512]
                    yk = "yacc%d_%d" % (tl, hf)
                    if gi == 0:
                        p.op("act", lambda e, py=py, dst=dst: e.activation(out=dst, in_=py[:], func=AF.Copy), reads=["ps%d" % bank], writes=[yk])
                    else:
                        p.op("dve", lambda e, py=py, dst=dst: e.tensor_tensor(out=dst, in0=py[:], in1=dst, op=ALU.add), reads=["ps%d" % bank, yk], writes=[yk])

        steps = [(gi, sg_) for gi in range(len(groups)) for sg_ in range(TGM // SG)]
        load(0)
        if len(groups) > 1:
            load(1)
        stepH(*steps[0])
        for si, (gi, sg_) in enumerate(steps):
            if si + 1 < len(steps):
                stepH(*steps[si + 1])
            stepY(gi, sg_)
            if sg_ == TGM // SG - 1 and gi + 2 < len(groups):
                load(gi + 2)
        p.pop()
        p.push()
        g2b = p.sbuf("g2b", [128, D], F32)
        fnw = p.sbuf("fnw", [128, D], F32)
        x1t = [p.sbuf("x1t%d" % i, [128, D], F32) for i in range(2)]
        tmp = [p.sbuf("tmp%d" % i, [128, D], F32) for i in range(2)]
        st = p.sbuf("st", [128, 4], F32)
        p.dma("sp", g2b[:], g.gbc_d[:, (2 + b) * D:(3 + b) * D], writes=["g2b"])
        p.dma("sp", fnw[:], g.final_w[0:1, :].partition_broadcast(128), writes=["fnw"])
        for tl in range(NT):
            i = tl % 2
            rows = slice(t0 + tl * 128, t0 + (tl + 1) * 128)
            p.dma("sp", x1t[i][:], g.x1d[b][rows, :], writes=["x1t%d" % i])
            p.op("dve", lambda e, i=i, tl=tl: e.tensor_tensor(out=tmp[i][:], in0=yacc[:, tl, :], in1=g2b[:], op=ALU.mult), reads=["g2b"], writes=["tmp%d" % i])
            p.op("pool", lambda e, i=i: e.tensor_tensor(out=tmp[i][:], in0=tmp[i][:], in1=x1t[i][:], op=ALU.add), reads=["tmp%d" % i, "x1t%d" % i], writes=["tmp%d" % i])
            p.op("act", lambda e, i=i: e.activation(out=x1t[i][:], in_=tmp[i][:], func=AF.Square, accum_out=st[:, 0:1]), reads=["tmp%d" % i], writes=["x1t%d" % i, "st"])
            p.op("act", lambda e: e.activation(out=st[:, 1:2], in_=st[:, 0:1], func=AF.Ln, scale=1.0 / D, bias=EPS), reads=["st"], writes=["st"])
            p.op("act", lambda e: e.activation(out=st[:, 2:3], in_=st[:, 1:2], func=AF.Exp, scale=-0.5), reads=["st"], writes=["st"])
            p.op("dve", lambda e, i=i: e.scalar_tensor_tensor(out=x1t[i][:], in0=tmp[i][:], scalar=st[:, 2:3], in1=fnw[:], op0=ALU.mult, op1=ALU.mult),
                 reads=["tmp%d" % i, "st", "fnw"], writes=["x1t%d" % i])
            p.dma("act", g.out[b][rows, :], x1t[i][:], reads=["x1t%d" % i], writes=["out"])
        p.pop()
        p.pop()
    return


def make_in_maps(inputs):
    x = np.asarray(inputs["x"], np.float32)
    c = np.asarray(inputs["c"], np.float32)
    ctx = np.asarray(inputs["ctx"], np.float32)
    c_ctx = np.asarray(inputs["c_ctx"], np.float32)
    hc = host_consts()
    shared = {
        "ada_w": np.ascontiguousarray(inputs["ada_w"][0]),
        "ada_bT": fm(inputs["ada_b"][0], 48),
        "ada_b": np.ascontiguousarray(inputs["ada_b"][0].reshape(1, -1)),
        "norm1_wT": fm(inputs["norm1_w"][0], 8),
        "norm2_wT": fm(inputs["norm2_w"][0], 8),
        "w_in": np.ascontiguousarray(inputs["w_in"][0]),
        "hy_conv_wT": np.ascontiguousarray(np.asarray(inputs["hy_conv_w"][0], np.float32).reshape(3, 24, 128).transpose(2, 1, 0)),
        "hy_conv_bT": fm(inputs["hy_conv_b"][0], 24),
        "ident": hc["ident"],
        "a_w2e": hc_a_w2e(inputs["gla_a_w2"][0]),
        "a_bT": np.ascontiguousarray(np.asarray(inputs["gla_a_b"][0], np.float32).reshape(2, 4, 128).transpose(2, 0, 1)),
        "gnwT": np.ascontiguousarray(np.asarray(inputs["gla_norm_w"][0], np.float32).reshape(4, 2, 128).transpose(2, 0, 1)),
        "maskf": hc["maskf"],
        "maskb": hc["maskb"],
        "proj_hy": np.ascontiguousarray(inputs["proj_hy"][0]), "proj_gla": np.ascontiguousarray(inputs["proj_gla"][0]),
        "w_out": np.ascontiguousarray(inputs["w_out"][0]), "router_w": np.ascontiguousarray(inputs["router_w"][0]),
        "router_b": np.ascontiguousarray(inputs["router_bias"][0].reshape(1, 64)),
        "final_w": np.ascontiguousarray(np.asarray(inputs["final_norm_w"], np.float32).reshape(1, D)),
        "exp_w1": np.ascontiguousarray(inputs["exp_w1"][0]), "exp_w3": np.ascontiguousarray(inputs["exp_w3"][0]),
        "exp_w2": np.ascontiguousarray(inputs["exp_w2"][0]),
        "sh_w1": np.ascontiguousarray(inputs["sh_w1"][0]), "sh_w3": np.ascontiguousarray(inputs["sh_w3"][0]),
        "sh_w2": np.ascontiguousarray(inputs["sh_w2"][0]),
        "sel": hc["sel"],
        "zext": hc["zext"], "text": hc["text"], "deltaT": hc["deltaT"],
        "hy_w1": np.ascontiguousarray(inputs["hy_w1"][0]), "hy_w2": np.ascontiguousarray(inputs["hy_w2"][0]),
        "hy_w3": np.ascontiguousarray(inputs["hy_w3"][0]),
        "hy_bf": np.ascontiguousarray(np.stack([inputs["hy_b1"][0], inputs["hy_b2"][0], inputs["hy_freq"][0][0], inputs["hy_freq"][0][1]], axis=1).astype(np.float32)),
        "hy_biasT": np.ascontiguousarray(np.asarray(inputs["hy_bias"][0], np.float32).reshape(2, 16, 64).transpose(2, 1, 0)),
        "tF64": hc["tF64"], "tG": hc["tG"], "tGp": hc["tGp"], "tC3": hc["tC3"],
    }
    maps = []
    for core in range(8):
        b0 = core * NB
        c3 = np.zeros((4, D), np.float32)
        c3[0] = c[b0]
        c3[1] = c[b0 + 1]
        c3[2] = c_ctx
        m = dict(shared)
        m["x"] = np.ascontiguousarray(x[b0:b0 + NB])
        m["ctx"] = np.ascontiguousarray(ctx[b0:b0 + NB])
        m["c3T"] = np.ascontiguousarray(c3.reshape(4, 8, 128).transpose(2, 1, 0))
        maps.append(m)
    return maps


def kernel(**inputs):
    nc = build_program()
    maps = make_in_maps(inputs)
    res = run_bass_kernel_spmd(nc, maps, core_ids=list(range(8)))
    return np.concatenate([np.asarray(r["out"]) for r in res.results], axis=0)
```

```python
import math
import numpy as np
import concourse.bass as bass
import concourse.mybir as mybir
from concourse.bass_utils import run_bass_kernel_spmd
from contextlib import ExitStack

F32 = mybir.dt.float32
BF16 = mybir.dt.bfloat16
I32 = mybir.dt.int32
ALU = mybir.AluOpType
AF = mybir.ActivationFunctionType
AX = mybir.AxisListType

ENGS = ("pe", "act", "dve", "pool", "sp")
N_DMA_SEMS = 48
SAME_ENGINE_NOSYNC = ("pe",)

D = 1024
L = 4096
CTX = 256
NB = 2
N_IN = 8224
EPS = 1e-6
C_K, C_V, C_A, C_Q, C_G, C_HY, C_GATE = 0, 512, 1536, 1568, 2080, 3104, 6176


class Prog:
    def __init__(self, nc):
        self.nc = nc
        self.es = ExitStack()
        self.stack = [self.es]
        self.streams = {e: [] for e in ENGS}
        self.cnt = {e: 0 for e in ENGS}
        self.esem = {}
        for e in ENGS:
            self.esem[e] = self.es.enter_context(nc.semaphore("sem_" + e))
        self.dsem = [self.es.enter_context(nc.semaphore("dsem%d" % i)) for i in range(N_DMA_SEMS)]
        self.dcnt = [0] * N_DMA_SEMS
        self.dnext = 0
        self.waited = {e: {} for e in ENGS}
        self.last_w = {}
        self.readers = {}
        self.n_ops = 0
        self.uid = 0

    def push(self):
        es = ExitStack()
        self.stack.append(es)

    def pop(self):
        self.barrier()
        self.stack.pop().close()

    def sbuf(self, name, shape, dtype):
        self.uid += 1
        return self.stack[-1].enter_context(self.nc.sbuf_tensor("%s_%d" % (name, self.uid), list(shape), dtype))

    def psum(self, name, shape, dtype):
        return self.stack[-1].enter_context(self.nc.psum_tensor(name, list(shape), dtype))

    def _deps(self, reads, writes):
        deps = []
        for k in reads:
            t = self.last_w.get(k)
            if t is not None:
                deps.append(t)
        for k in writes:
            t = self.last_w.get(k)
            if t is not None:
                deps.append(t)
            deps.extend(self.readers.get(k, ()))
        return deps

    def _update(self, tok, reads, writes):
        for k in reads:
            self.readers.setdefault(k, []).append(tok)
        for k in writes:
            self.last_w[k] = tok
            self.readers[k] = []

    def _waits(self, eng, deps):
        need = {}
        own = "e_" + eng
        for (s, v, sid) in deps:
            if sid == own and eng in SAME_ENGINE_NOSYNC:
                continue
            if need.get(sid, (None, 0))[1] < v:
                need[sid] = (s, v)
        out = []
        w = self.waited[eng]
        for sid, (s, v) in need.items():
            if w.get(sid, 0) >= v:
                continue
            w[sid] = v
            out.append((s, v))
        return out

    def op(self, eng, fn, reads=(), writes=(), extra=()):
        deps = self._deps(reads, writes) + list(extra)
        waits = self._waits(eng, deps)
        self.cnt[eng] += 1
        tok = (self.esem[eng], self.cnt[eng], "e_" + eng)
        self.streams[eng].append((waits, fn, (self.esem[eng], 1)))
        self._update(tok, reads, writes)
        self.n_ops += 1
        return tok

    def dma(self, eng, out, in_, reads=(), writes=(), extra=(), **kw):
        i = self.dnext
        self.dnext = (self.dnext + 1) % N_DMA_SEMS
        deps = self._deps(reads, writes) + list(extra)
        if self.dcnt[i] > 0:
            deps.append((self.dsem[i], self.dcnt[i], "d%d" % i))
        waits = self._waits(eng, deps)
        self.dcnt[i] += 16
        tok = (self.dsem[i], self.dcnt[i], "d%d" % i)

        def fn(e, out=out, in_=in_, kw=kw):
            return e.dma_start(out=out, in_=in_, **kw)

        self.streams[eng].append((waits, fn, (self.dsem[i], 16)))
        self._update(tok, reads, writes)
        self.n_ops += 1
        return tok

    def barrier(self):
        toks = [(self.esem[e], self.cnt[e], "e_" + e) for e in ENGS if self.cnt[e] > 0]
        toks += [(self.dsem[i], self.dcnt[i], "d%d" % i) for i in range(N_DMA_SEMS) if self.dcnt[i] > 0]
        for e in ENGS:
            need = {}
            for (s, v, sid) in toks:
                need[sid] = (s, v)
            w = self.waited[e]
            waits = []
            for sid, (s, v) in need.items():
                if sid == "e_" + e:
                    continue
                if w.get(sid, 0) >= v:
                    continue
                w[sid] = v
                waits.append((s, v))
            if waits:
                self.streams[e].append((waits, None, None))
        self.last_w.clear()
        self.readers.clear()

    def emit(self):
        nc = self.nc
        streams = self.streams

        def run(e, name):
            for (waits, fn, inc) in streams[name]:
                for (s, v) in waits:
                    e.wait_ge(s, v)
                if fn is not None:
                    ins = fn(e)
                    if inc is not None:
                        ins.then_inc(inc[0], inc[1])

        with nc.Block() as block:
            @block.tensor
            def _(e):
                run(e, "pe")

            @block.scalar
            def _(e):
                run(e, "act")

            @block.vector
            def _(e):
                run(e, "dve")

            @block.gpsimd
            def _(e):
                run(e, "pool")

            @block.sync
            def _(e):
                run(e, "sp")

    def close(self):
        self.es.close()


def host_consts():
    c = {}
    c["ident"] = np.eye(128, dtype=np.float32)
    j = np.arange(128)[:, None]
    i = np.arange(128)[None, :]
    c["maskf"] = (j <= i).astype(np.float32)
    c["maskb"] = (j > i).astype(np.float32)
    sel = np.zeros((64, 64, 128), np.float32)
    for e in range(64):
        sel[e, e, :] = 1.0
    c["sel"] = sel
    Lh = L
    t = np.linspace(0.0, 1.0, Lh, dtype=np.float32)
    w = (2.0 * math.pi * np.arange(Lh, dtype=np.float32) / Lh).astype(np.float32)
    f = np.linspace(1e-4, 15.0, 16, dtype=np.float32)
    z = np.concatenate([t[:, None], np.cos(f[None, :] * w[:, None]), -np.sin(f[None, :] * w[:, None])], axis=1).astype(np.float32)
    zext = np.zeros((2 * Lh, 33), np.float32)
    zext[:Lh] = z
    zext[Lh + 1:] = z[:0:-1]
    text = np.zeros((2 * Lh,), np.float32)
    text[:Lh] = t
    text[Lh] = 1.0e4
    text[Lh + 1:] = t[:0:-1]
    c["zext"] = np.ascontiguousarray(zext.T)
    c["text"] = text.reshape(1, -1)
    max_decay = math.log(1e-2) / 0.3
    min_decay = math.log(1e-2) / 1.5
    deltas = np.abs(np.linspace(min_decay, max_decay, 1024, dtype=np.float32))
    c["deltaT"] = np.ascontiguousarray(deltas.reshape(8, 128).T)
    N = 8192
    n1 = np.arange(64, dtype=np.float64)[:, None]
    k1 = np.arange(64, dtype=np.float64)[None, :]
    ang = 2 * np.pi * n1 * k1 / 64
    c["tF64"] = np.concatenate([np.cos(ang), -np.sin(ang), np.sin(ang)], axis=1).astype(np.float32)
    n2 = np.arange(128, dtype=np.float64)[:, None, None]
    k1 = np.arange(64, dtype=np.float64)[None, :, None]
    k2 = np.arange(128, dtype=np.float64)[None, None, :]
    ang = 2 * np.pi * n2 * (k1 + 64 * k2) / N
    c["tG"] = np.stack([np.cos(ang), -np.sin(ang)], axis=2).astype(np.float32)
    kl = np.arange(64, dtype=np.float64)[:, None, None]
    ta = np.arange(128, dtype=np.float64)[None, :, None]
    tb = np.arange(32, dtype=np.float64)[None, None, :]
    ang = 2 * np.pi * (ta + 128 * tb) * kl / N
    c["tGp"] = (np.concatenate([np.cos(ang), -np.sin(ang)], axis=0) / N).astype(np.float32)
    kh = np.arange(128, dtype=np.float64)[:, None]
    ta = np.arange(128, dtype=np.float64)[None, :]
    ang = 2 * np.pi * ta * kh / 128
    c["tC3"] = np.stack([np.cos(ang), np.sin(ang), -np.sin(ang)], axis=1).astype(np.float32)
    return c


def hc_a_w2e(a_w2):
    o = np.zeros((32, 2, 512), np.float32)
    o[0:16, 0, :] = a_w2[0]
    o[16:32, 1, :] = a_w2[1]
    return o


def fm(v, nchunk):
    return np.ascontiguousarray(np.asarray(v, np.float32).reshape(nchunk, 128).T)


class K:
    pass


def build_program(dbg=()):
    nc = bass.Bass("TRN2", target_bir_lowering=False)
    dbg = set(dbg)

    def din(name, shape, dt=F32):
        return nc.dram_tensor(name, list(shape), dt, kind="ExternalInput").ap()

    def dscr(name, shape, dt=F32):
        kind = "ExternalOutput" if name in dbg else "Internal"
        return nc.dram_tensor(name, list(shape), dt, kind=kind).ap()

    g = K()
    g.nc = nc
    g.x = din("x", [NB, L, D])
    g.ctx = din("ctx", [NB, CTX, D])
    g.c3T = din("c3T", [128, 8, 4])
    g.ada_w = din("ada_w", [D, 6 * D])
    g.ada_bT = din("ada_bT", [128, 48])
    g.ada_b = din("ada_b", [1, 6 * D])
    g.norm1_wT = din("norm1_wT", [128, 8])
    g.norm2_wT = din("norm2_wT", [128, 8])
    g.w_in = din("w_in", [D, N_IN])
    g.hy_conv_wT = din("hy_conv_wT", [128, 24, 3])
    g.hy_conv_bT = din("hy_conv_bT", [128, 24])
    g.ident = din("ident", [128, 128])
    g.a_w2e = din("a_w2e", [32, 2, 512])
    g.a_bT = din("a_bT", [128, 2, 4])
    g.gnwT = din("gnwT", [128, 4, 2])
    g.maskf = din("maskf", [128, 128])
    g.maskb = din("maskb", [128, 128])
    g.proj_hy = din("proj_hy", [D, D])
    g.proj_gla = din("proj_gla", [D, D])
    g.w_out = din("w_out", [D, D])
    g.router_w = din("router_w", [D, 64])
    g.router_b = din("router_b", [1, 64])
    g.final_w = din("final_w", [1, D])
    g.exp_w1 = din("exp_w1", [64, D, 256])
    g.exp_w3 = din("exp_w3", [64, D, 256])
    g.exp_w2 = din("exp_w2", [64, 256, D])
    g.sh_w1 = din("sh_w1", [D, 256])
    g.sh_w3 = din("sh_w3", [D, 256])
    g.sh_w2 = din("sh_w2", [256, D])
    g.sel = din("sel", [64, 64, 128])
    g.zext = din("zext", [33, 8192])
    g.text = din("text", [1, 8192])
    g.deltaT = din("deltaT", [128, 8])
    g.hy_w1 = din("hy_w1", [33, 64])
    g.hy_w2 = din("hy_w2", [64, 64])
    g.hy_w3 = din("hy_w3", [64, 4096])
    g.hy_bf = din("hy_bf", [64, 4])
    g.hy_biasT = din("hy_biasT", [64, 16, 2])
    g.tF64 = din("tF64", [64, 192])
    g.tG = din("tG", [128, 64, 2, 128])
    g.tGp = din("tGp", [128, 128, 32])
    g.tC3 = din("tC3", [128, 3, 128])
    g.out = nc.dram_tensor("out", [NB, L, D], F32, kind="ExternalOutput").ap()
    g.uhy = [dscr("uhy%d" % b, [3, D, L], BF16) for b in range(NB)]
    g.gate = [dscr("gate%d" % b, [2 * D, L], BF16) for b in range(NB)]
    g.sg = [dscr("sg%d" % b, [D, L], BF16) for b in range(NB)]
    g.kT = [dscr("kT%d" % b, [512, L], BF16) for b in range(NB)]
    g.qT = [dscr("qT%d" % b, [512, L], BF16) for b in range(NB)]
    g.uaT = [dscr("uaT%d" % b, [32, L], F32) for b in range(NB)]
    g.v = [dscr("v%d" % b, [L, D], BF16) for b in range(NB)]
    g.ckT = [dscr("ckT%d" % b, [512, CTX], BF16) for b in range(NB)]
    g.cuaT = [dscr("cuaT%d" % b, [32, CTX], F32) for b in range(NB)]
    g.cv = [dscr("cv%d" % b, [CTX, D], BF16) for b in range(NB)]
    g.yglaT = [dscr("yglaT%d" % b, [D, L], BF16) for b in range(NB)]
    g.kernT = [dscr("kernT%d" % o, [D, 8192], BF16) for o in range(2)]
    g.sspec = [dscr("sspec%d" % o, [16, 128, 8192], BF16) for o in range(2)]
    g.z1T = [dscr("z1T%d" % b, [D, L], BF16) for b in range(NB)]
    g.yhyT = [dscr("yhyT%d" % b, [D, L], BF16) for b in range(NB)]
    g.x1d = [dscr("x1d%d" % b, [L, D], F32) for b in range(NB)]
    g.xn2T = [dscr("xn2T%d" % b, [D, L], BF16) for b in range(NB)]
    g.combT = [dscr("combT%d" % b, [64, L], BF16) for b in range(NB)]
    g.dbg_sc = dscr("dbg_sc", [L, 64], F32)
    g.gbc_d = dscr("gbc_d", [128, 4 * D])
    g.dbg_state = dscr("dbg_state", [128, NB * 2 * 4 * 256])
    g.dbg_h2 = dscr("dbg_h2", [64, 8192], BF16)
    g.dbg_h1 = dscr("dbg_h1", [64, 4096], F32)
    g.dbg_mt = dscr("dbg_mt", [64, 4096], F32)
    g.dbg_kern = dscr("dbg_kern", [128, 8192], F32)
    g.dbg_st = dscr("dbg_st", [128, 4], F32)
    g.dbg_modT = dscr("dbg_modT", [128, 48 * 4])
    g.dbg_gb = dscr("dbg_gb", [128, 4 * D])
    g.dbg_xnT = dscr("dbg_xnT", [128, 8 * L], BF16)

    p = Prog(nc)
    g.p = p
    g.ident_f = p.sbuf("ident_f", [128, 128], F32)
    g.ident_b = p.sbuf("ident_b", [128, 128], BF16)
    g.modT = p.sbuf("modT", [128, 48, 4], F32)
    g.scale1T = p.sbuf("scale1T", [128, 8, 4], F32)
    g.scale2T = p.sbuf("scale2T", [128, 8, 4], F32)
    g.ps = [p.psum("ps%d" % i, [128, 512], F32) for i in range(8)]

    p.dma("sp", g.ident_f[:], g.ident, writes=["ident_f"])
    p.op("dve", lambda e: e.tensor_copy(out=g.ident_b[:], in_=g.ident_f[:]), reads=["ident_f"], writes=["ident_b"])

    phase0(g)
    if "stop0" in dbg:
        p.dma("sp", g.dbg_modT, g.modT[:].rearrange("p a b -> p (a b)"), reads=["modT"], writes=["o1"])
    else:
        p.push()
        g.s0 = p.sbuf("s0", [128, NB, 2, 4, 256], F32)
        for b in range(NB):
            if "noctx" in dbg:
                break
            phase_proj(g, b, ctx=True)
            phase_gla(g, b, ctx=True)
        if "stop1" in dbg:
            p.dma("sp", g.dbg_state, g.s0[:].rearrange("p a b c d -> p (a b c d)"), reads=["s0"], writes=["o3"])
        for b in range(NB):
            if "stop1" in dbg:
                break
            if "noproj" not in dbg:
                phase_proj(g, b, ctx=False)
            if "stop3" in dbg:
                break
            if "nogla" not in dbg:
                phase_gla(g, b, ctx=False)
            if "stop4" in dbg:
                break
        p.pop()
        if "stop1" not in dbg and "stop3" not in dbg and "stop4" not in dbg:
            if "nohy" not in dbg:
                phase_filter(g)
                if "stop5" not in dbg:
                    phase_hyena(g, dbg)
            if not ({"stop5", "stop6", "stop7"} & dbg):
                for b in range(NB):
                    if "nomerge" not in dbg:
                        phase_merge(g, b, dbg)
                    if "stop8" in dbg:
                        break
                    phase_moe(g, b, dbg)
                    if "stop9" in dbg:
                        break
    p.barrier()
    p.emit()
    p.close()
    return nc


def phase0(g):
    p = g.p
    p.push()
    c3T = p.sbuf("c3T", [128, 8, 4], F32)
    sT = p.sbuf("sT", [128, 8, 4], F32)
    sTrep = p.sbuf("sTrep", [128, 8, 2, 128], F32)
    ada_bT = p.sbuf("ada_bT", [128, 48], F32)
    n1 = p.sbuf("n1", [128, 8], F32)
    n2 = p.sbuf("n2", [128, 8], F32)
    abb = p.sbuf("abb", [128, 2, D], F32)
    aw = [p.sbuf("aw%d" % i, [128, 8, 512], F32) for i in range(2)]
    g.gbc = p.sbuf("gbc", [128, 4, D], F32)
    p.dma("sp", c3T[:], g.c3T, writes=["c3T"])
    p.dma("sp", ada_bT[:], g.ada_bT, writes=["ada_bT"])
    p.dma("sp", n1[:], g.norm1_wT, writes=["n1"])
    p.dma("sp", n2[:], g.norm2_wT, writes=["n2"])
    p.dma("sp", abb[:, 0, :], g.ada_b[0:1, 2 * D:3 * D].partition_broadcast(128), writes=["abb0"])
    p.dma("sp", abb[:, 1, :], g.ada_b[0:1, 5 * D:6 * D].partition_broadcast(128), writes=["abb1"])
    p.op("act", lambda e: e.activation(out=sT[:], in_=c3T[:], func=AF.Silu), reads=["c3T"], writes=["sT"])
    for b in range(2):
        for k in range(8):
            p.op("dve", lambda e, b=b, k=k: e.tensor_copy(out=sTrep[:, k, b, :], in_=sT[:, k, b:b + 1].to_broadcast([128, 128])),
                 reads=["sT"], writes=["sTrep"])
    psT = g.ps[0]
    aw_view = g.ada_w.rearrange("(k p) n -> p k n", p=128)
    for ci in range(12):
        a = aw[ci % 2]
        ak = "aw%d" % (ci % 2)
        p.dma("sp", a[:], aw_view[:, :, ci * 512:(ci + 1) * 512], writes=[ak])
        for jj in range(4):
            j = ci * 4 + jj
            for k in range(8):
                p.op("pe", lambda e, a=a, jj=jj, j=j, k=k: e.matmul(psT[:, j * 4:(j + 1) * 4], lhsT=a[:, k, jj * 128:(jj + 1) * 128],
                                                                    rhs=sT[:, k, :], start=(k == 0), stop=(k == 7)),
                     reads=[ak, "sT"], writes=["ps0"])
        if ci in (4, 5, 10, 11):
            which = 0 if ci < 6 else 1
            half = ci % 2 if ci < 6 else (ci - 10)
            for b in range(2):
                pb = g.ps[1 + b]
                for k in range(8):
                    p.op("pe", lambda e, a=a, b=b, k=k, pb=pb: e.matmul(pb[:], lhsT=sTrep[:, k, b, :], rhs=a[:, k, :], start=(k == 0), stop=(k == 7)),
                         reads=[ak, "sTrep"], writes=["ps%d" % (1 + b)])
                p.op("dve", lambda e, b=b, which=which, half=half, pb=pb: e.tensor_tensor(
                    out=g.gbc[:, which * 2 + b, half * 512:(half + 1) * 512], in0=pb[:], in1=abb[:, which, half * 512:(half + 1) * 512], op=ALU.add),
                    reads=["ps%d" % (1 + b), "abb%d" % which], writes=["gbc"])
    p.op("dve", lambda e: e.tensor_tensor(out=g.modT[:], in0=psT[:, 0:192].rearrange("p (a b) -> p a b", b=4),
                                          in1=ada_bT[:].unsqueeze(2).to_broadcast([128, 48, 4]), op=ALU.add),
         reads=["ps0", "ada_bT"], writes=["modT"])
    p.op("dve", lambda e: e.scalar_tensor_tensor(out=g.scale1T[:], in0=g.modT[:, 8:16, :], scalar=1.0,
                                                 in1=n1[:].unsqueeze(2).to_broadcast([128, 8, 4]), op0=ALU.add, op1=ALU.mult),
         reads=["modT", "n1"], writes=["scale1T"])
    p.op("dve", lambda e: e.scalar_tensor_tensor(out=g.scale2T[:], in0=g.modT[:, 32:40, :], scalar=1.0,
                                                 in1=n2[:].unsqueeze(2).to_broadcast([128, 8, 4]), op0=ALU.add, op1=ALU.mult),
         reads=["modT", "n2"], writes=["scale2T"])
    p.dma("sp", g.gbc_d, g.gbc[:].rearrange("p a b -> p (a b)"), reads=["gbc"], writes=["gbc_d"])
    p.pop()


def norm_transpose(g, src, T, r, xnT, scaleT, shift_j0, xin, xsb, junk, stat, tagbase):
    p = g.p
    NTL = T // 128

    def st1(t):
        s = t % 2
        xi, xs = xin[s], xsb[s]
        sk = "stat%d" % s
        p.dma("sp", xi[:], src[t * 128:(t + 1) * 128, :], writes=["xin%d" % s])
        p.op("act", lambda e: e.activation(out=junk[:], in_=xi[:], func=AF.Square, accum_out=stat[:, 4 * s:4 * s + 1]),
             reads=["xin%d" % s], writes=["junk", sk])
        p.op("act", lambda e: e.activation(out=stat[:, 4 * s + 1:4 * s + 2], in_=stat[:, 4 * s:4 * s + 1], func=AF.Ln, scale=1.0 / D, bias=EPS), reads=[sk], writes=[sk])
        p.op("act", lambda e: e.activation(out=stat[:, 4 * s + 2:4 * s + 3], in_=stat[:, 4 * s + 1:4 * s + 2], func=AF.Exp, scale=-0.5), reads=[sk], writes=[sk])
        p.op("dve", lambda e: e.tensor_scalar(out=xs[:], in0=xi[:], scalar1=stat[:, 4 * s + 2:4 * s + 3], scalar2=None, op0=ALU.mult),
             reads=["xin%d" % s, sk], writes=["xsb%d" % s])

    def st2(t):
        s = t % 2
        xs = xsb[s]
        pst = g.ps[6 + s]
        pk = "ps%d" % (6 + s)
        pv = pst[:].bitcast(BF16)
        for k in range(8):
            p.op("pe", lambda e, k=k: e.transpose(pv[:, k * 128:(k + 1) * 128], xs[:, k * 128:(k + 1) * 128], g.ident_b[:]),
                 reads=["xsb%d" % s, "ident_b"], writes=[pk])
        for k in range(8):
            p.op("act", lambda e, k=k: e.activation(out=xnT[:, k, t * 128:(t + 1) * 128], in_=pv[:, k * 128:(k + 1) * 128],
                                                    func=AF.Identity, scale=scaleT[:, k, r:r + 1],
                                                    bias=g.modT[:, shift_j0 + k, r:r + 1]),
                 reads=[pk, "modT", "scale"], writes=[tagbase])

    st1(0)
    for t in range(NTL):
        if t + 1 < NTL:
            st1(t + 1)
        st2(t)


def phase_proj(g, b, ctx):
    p = g.p
    T = CTX if ctx else L
    r = 2 if ctx else b
    src = g.ctx[b] if ctx else g.x[b]
    p.push()
    xnT = p.sbuf("xnT", [128, 8, T], BF16)
    xin = [p.sbuf("xin%d" % i, [128, D], F32) for i in range(2)]
    xsb = [p.sbuf("xsb%d" % i, [128, D], BF16) for i in range(2)]
    junk = p.sbuf("junk", [128, D], F32)
    stat = p.sbuf("stat", [128, 8], F32)
    wst = [p.sbuf("wst%d" % i, [128, 8, 128], F32) for i in range(2)]
    wbf = [p.sbuf("wbf%d" % i, [128, 8, 128], BF16) for i in range(2)]
    wvst = [p.sbuf("wvst%d" % i, [128, 8, 512], F32) for i in range(2)]
    wvbf = [p.sbuf("wvbf%d" % i, [128, 8, 512], BF16) for i in range(2)]
    ost = [p.sbuf("ost%d" % i, [128, 512], BF16) for i in range(4)]
    osf = [p.sbuf("osf%d" % i, [128, 512], F32) for i in range(2)]
    cw = p.sbuf("cw", [128, 24, 3], F32)
    cb = p.sbuf("cb", [128, 24], F32)
    p.dma("sp", cw[:], g.hy_conv_wT, writes=["cw"])
    p.dma("sp", cb[:], g.hy_conv_bT, writes=["cb"])
    norm_transpose(g, src, T, r, xnT, g.scale1T, 0, xin, xsb, junk, stat, "xnT")
    if (not ctx) and b == 0:
        p.dma("act", g.dbg_xnT, xnT[:].rearrange("p a b -> p (a b)"), reads=["xnT"], writes=["dbgx"])

    TG = 512 if T >= 512 else T
    ntg = T // TG
    w_view = g.w_in.rearrange("(k p) n -> p k n", p=128)
    if ctx:
        chunks = [("k", C_K + i * 128, 128, i) for i in range(4)] + [("a", C_A, 32, 0)]
    else:
        chunks = ([("k", C_K + i * 128, 128, i) for i in range(4)] + [("a", C_A, 32, 0)] +
                  [("q", C_Q + i * 128, 128, i) for i in range(4)] +
                  [("g", C_G + i * 128, 128, i) for i in range(8)] +
                  [("hy", C_HY + i * 128, 128, i) for i in range(24)] +
                  [("gate", C_GATE + i * 128, 128, i) for i in range(16)])
    if not ctx:
        hy = [c for c in chunks if c[0] == "hy"]
        oth = [c for c in chunks if c[0] != "hy"]
        chunks = []
        while hy or oth:
            if oth:
                chunks.append(oth.pop(0))
            if hy:
                chunks.append(hy.pop(0))
    n_ep = 0
    for ci, (kind, c0, M, idx) in enumerate(chunks):
        s = ci % 2
        ws_, wb_ = wst[s], wbf[s]
        p.dma("sp", ws_[:, :, 0:M], w_view[:, :, c0:c0 + M], writes=["wst%d" % s])
        p.op("pool", lambda e, ws_=ws_, wb_=wb_, M=M: e.tensor_copy(out=wb_[:, :, 0:M], in_=ws_[:, :, 0:M]),
             reads=["wst%d" % s], writes=["wbf%d" % s])
        for tg in range(ntg):
            bank = (ci * ntg + tg) % 4
            ps = g.ps[bank]
            pk = "ps%d" % bank
            ts = slice(tg * TG, (tg + 1) * TG)
            for k in range(8):
                p.op("pe", lambda e, ps=ps, wb_=wb_, M=M, k=k, ts=ts: e.matmul(ps[0:M, 0:TG], lhsT=wb_[:, k, 0:M], rhs=xnT[:, k, ts],
                                                                              start=(k == 0), stop=(k == 7)),
                     reads=["wbf%d" % s, "xnT"], writes=[pk])
            n_ep += 1
            oi = n_ep % 4
            o = ost[oi]
            ok = "ost%d" % oi
            if kind in ("k", "q"):
                dst = (g.ckT[b] if ctx else g.kT[b]) if kind == "k" else g.qT[b]
                sc = 1.0 if kind == "k" else 128 ** -0.5
                p.op("act", lambda e, o=o, ps=ps, sc=sc: e.activation(out=o[:, 0:TG], in_=ps[:, 0:TG], func=AF.Copy, scale=sc), reads=[pk], writes=[ok])
                p.dma("act", dst[idx * 128:(idx + 1) * 128, ts], o[:, 0:TG], reads=[ok], writes=["scr"])
            elif kind == "a":
                of = osf[n_ep % 2]
                ofk = "osf%d" % (n_ep % 2)
                p.op("dve", lambda e, of=of, ps=ps: e.tensor_copy(out=of[0:32, 0:TG], in_=ps[0:32, 0:TG]), reads=[pk], writes=[ofk])
                dst = g.cuaT[b] if ctx else g.uaT[b]
                p.dma("act", dst[:, ts], of[0:32, 0:TG], reads=[ofk], writes=["scr"])
            elif kind == "g":
                p.op("act", lambda e, o=o, ps=ps: e.activation(out=o[:], in_=ps[:], func=AF.Silu), reads=[pk], writes=[ok])
                p.dma("act", g.sg[b][idx * 128:(idx + 1) * 128, ts], o[:], reads=[ok], writes=["scr"])
            elif kind == "gate":
                p.op("act", lambda e, o=o, ps=ps: e.activation(out=o[:], in_=ps[:], func=AF.Sigmoid), reads=[pk], writes=[ok])
                p.dma("act", g.gate[b][idx * 128:(idx + 1) * 128, ts], o[:], reads=[ok], writes=["scr"])
            elif kind == "hy":
                of = osf[n_ep % 2]
                ofk = "osf%d" % (n_ep % 2)
                p.op("act", lambda e, of=of, ps=ps, idx=idx: e.activation(out=of[:], in_=ps[:], func=AF.Identity, scale=cw[:, idx, 1:2], bias=cb[:, idx:idx + 1]),
                     reads=[pk, "cw", "cb"], writes=[ofk])
                ofv = of[:].rearrange("p (r c) -> p r c", c=64)
                psv = ps[:].rearrange("p (r c) -> p r c", c=64)
                p.op("dve", lambda e, ofv=ofv, psv=psv, idx=idx: e.scalar_tensor_tensor(out=ofv[:, :, 1:64], in0=psv[:, :, 0:63], scalar=cw[:, idx, 0:1],
                                                                                    in1=ofv[:, :, 1:64], op0=ALU.mult, op1=ALU.add),
                     reads=[pk, ofk, "cw"], writes=[ofk])
                p.op("dve", lambda e, o=o, ofv=ofv, psv=psv, idx=idx: e.scalar_tensor_tensor(
                    out=o[:].rearrange("p (r c) -> p r c", c=64)[:, :, 0:63], in0=psv[:, :, 1:64], scalar=cw[:, idx, 2:3],
                    in1=ofv[:, :, 0:63], op0=ALU.mult, op1=ALU.add),
                    reads=[pk, ofk, "cw"], writes=[ok])
                p.op("dve", lambda e, o=o, ofv=ofv: e.tensor_copy(out=o[:].rearrange("p (r c) -> p r c", c=64)[:, :, 63:64], in_=ofv[:, :, 63:64]),
                     reads=[ofk], writes=[ok])
                part, cc = idx // 8, idx % 8
                p.dma("act", g.uhy[b][part, cc * 128:(cc + 1) * 128, ts], o[:], reads=[ok], writes=["scr"])
    for vc in range(2):
        s = vc % 2
        p.dma("sp", wvst[s][:], w_view[:, :, C_V + vc * 512:C_V + (vc + 1) * 512], writes=["wvst%d" % s])
        p.op("pool", lambda e, s=s: e.tensor_copy(out=wvbf[s][:], in_=wvst[s][:]), reads=["wvst%d" % s], writes=["wvbf%d" % s])
        for t in range(T // 128):
            bank = 4 + (t % 2)
            ps = g.ps[bank]
            pk = "ps%d" % bank
            for k in range(8):
                p.op("pe", lambda e, ps=ps, s=s, k=k, t=t: e.matmul(ps[:], lhsT=xnT[:, k, t * 128:(t + 1) * 128], rhs=wvbf[s][:, k, :],
                                                                   start=(k == 0), stop=(k == 7)),
                     reads=["wvbf%d" % s, "xnT"], writes=[pk])
            n_ep += 1
            oi = n_ep % 4
            o = ost[oi]
            ok = "ost%d" % oi
            if t % 2 == 0:
                p.op("act", lambda e, o=o, ps=ps: e.activation(out=o[:], in_=ps[:], func=AF.Copy), reads=[pk], writes=[ok])
            else:
                p.op("dve", lambda e, o=o, ps=ps: e.tensor_copy(out=o[:], in_=ps[:]), reads=[pk], writes=[ok])
            dst = g.cv[b] if ctx else g.v[b]
            p.dma("act", dst[t * 128:(t + 1) * 128, vc * 512:(vc + 1) * 512], o[:], reads=[ok], writes=["scr"])
    p.pop()


def phase_gla(g, b, ctx):
    p = g.p
    T = CTX if ctx else L
    NCH = T // 128
    kT_src = g.ckT[b] if ctx else g.kT[b]
    ua_src = g.cuaT[b] if ctx else g.uaT[b]
    v_src = g.cv[b] if ctx else g.v[b]
    p.push()
    aw2f = p.sbuf("aw2f", [32, 2, 512], F32)
    aw2 = p.sbuf("aw2", [32, 2, 512], BF16)
    abT = p.sbuf("abT", [128, 2, 4], F32)
    nabT = p.sbuf("nabT", [128, 2, 4], F32)
    gnw = p.sbuf("gnw", [128, 4, 2], F32)
    mkf = p.sbuf("mkf", [128, 128], F32)
    mkb = p.sbuf("mkb", [128, 128], F32)
    ones_b = p.sbuf("ones_b", [128, 128], BF16)
    m01 = p.sbuf("m01", [128, T], BF16)
    uaf = p.sbuf("uaf", [32, 256], F32)
    uab = p.sbuf("uab", [32, T], BF16)
    A1 = p.sbuf("A1", [128, T], F32)
    A2 = p.sbuf("A2", [128, T], F32)
    E1 = p.sbuf("E1", [128, T], BF16)
    kT = p.sbuf("kT", [128, T], BF16)
    kf = p.sbuf("kf", [128, T], BF16)
    kb = p.sbuf("kb", [128, T], BF16)
    vh = p.sbuf("vh", [128, NCH, 256], BF16)
    Dq = p.sbuf("Dq", [128, NCH], F32)
    Db = p.sbuf("Db", [128, NCH], F32)
    S32 = p.sbuf("S32", [128, 256], F32)
    Stmp = p.sbuf("Stmp", [128, 256], F32)
    ktk_f = p.sbuf("ktk_f", [128, NCH, 128], BF16)
    ktk_b = p.sbuf("ktk_b", [128, NCH, 128], BF16)
    if not ctx:
        qT = p.sbuf("qT", [128, T], BF16)
        qf = p.sbuf("qf", [128, T], BF16)
        qb = p.sbuf("qb", [128, T], BF16)
        sg = p.sbuf("sg", [128, 2, T], BF16)
        Rbf = p.sbuf("Rbf", [128, NCH, 256], BF16)
        Sbf = [p.sbuf("Sbf%d" % i, [128, 256], BF16) for i in range(2)]
        Acm = [p.sbuf("Acm%d" % i, [128, 128], BF16) for i in range(2)]
        Atmp = p.sbuf("Atmp", [128, 128], F32)
        sq2 = [p.sbuf("sq%d" % i, [128, 2, 128], BF16) for i in range(2)]
        Atm2 = p.sbuf("Atm2", [128, 128], F32)
        rs = p.sbuf("rs", [128, 128], F32)
        ytmp = p.sbuf("ytmp", [128, 2, 128], F32)
    p.dma("sp", aw2f[:], g.a_w2e, writes=["aw2f"])
    p.dma("sp", abT[:], g.a_bT, writes=["abT"])
    p.dma("sp", gnw[:], g.gnwT, writes=["gnw"])
    p.dma("sp", mkf[:], g.maskf, writes=["mkf"])
    p.dma("sp", mkb[:], g.maskb, writes=["mkb"])
    p.op("dve", lambda e: e.tensor_copy(out=aw2[:], in_=aw2f[:]), reads=["aw2f"], writes=["aw2"])
    p.op("dve", lambda e: e.tensor_scalar(out=nabT[:], in0=abT[:], scalar1=-1.0, scalar2=None, op0=ALU.mult), reads=["abT"], writes=["nabT"])
    for i in range(T // 256):
        p.dma("sp", uaf[:], ua_src[:, i * 256:(i + 1) * 256], writes=["uaf"])
        p.op("dve", lambda e, i=i: e.tensor_copy(out=uab[:, i * 256:(i + 1) * 256], in_=uaf[:]), reads=["uaf"], writes=["uab"])
    p.op("pool", lambda e: e.memset(ones_b[:], 1.0), writes=["ones_b"])
    p.op("pool", lambda e: e.memset(m01[:], 1.0), writes=["m01"])
    p.op("pool", lambda e: e.memset(m01[:].rearrange("p (c t) -> p c t", t=128)[:, :, 0:1], 0.0), writes=["m01"])
    TG = 512 if T >= 512 else T

    def softplus_scan(h, d, dst_sp, dst_B):
        for tg in range(T // TG):
            ts = slice(tg * TG, (tg + 1) * TG)
            bank = tg % 2
            ps = g.ps[bank]
            pk = "ps%d" % bank
            p.op("pe", lambda e, ps=ps, ts=ts: e.matmul(ps[:, 0:TG], lhsT=aw2[:, d, h * 128:(h + 1) * 128], rhs=uab[:, ts], start=True, stop=True),
                 reads=["aw2", "uab"], writes=[pk])
            p.op("act", lambda e, ps=ps, ts=ts: e.activation(out=dst_sp[:, ts], in_=ps[:, 0:TG], func=AF.Exp, scale=-1.0, bias=nabT[:, d, h:h + 1]),
                 reads=[pk, "nabT"], writes=["A1"])
        p.op("act", lambda e: e.activation(out=dst_sp[:], in_=dst_sp[:], func=AF.Ln, scale=1.0, bias=1.0), reads=["A1"], writes=["A1"])
        p.op("dve", lambda e: e.tensor_tensor_scan(out=dst_B[:], data0=m01[:], data1=dst_sp[:], initial=0.0, op0=ALU.mult, op1=ALU.add),
             reads=["A1", "m01"], writes=["A2"])

    def do_head(h):
        hk = slice(h * 128, (h + 1) * 128)
        p.dma("sp", kT[:], kT_src[hk, :], writes=["kT"])
        p.dma("sp", vh[:], v_src.rearrange("(c p) n -> p c n", p=128)[:, :, h * 256:(h + 1) * 256], writes=["vh"])
        if not ctx:
            p.dma("sp", qT[:], g.qT[b][hk, :], writes=["qT"])
            p.dma("sp", sg[:], g.sg[b][h * 256:(h + 1) * 256, :].rearrange("(a p) t -> p a t", p=128), writes=["sg"])
        softplus_scan(h, 0, A1, A2)
        A2c = A2[:].rearrange("p (c t) -> p c t", t=128)
        p.op("act", lambda e: e.activation(out=Dq[:], in_=A2c[:, :, 127], func=AF.Exp, scale=-1.0 / 16), reads=["A2"], writes=["Dq"])
        p.op("act", lambda e: e.activation(out=E1[:], in_=A2[:], func=AF.Exp, scale=1.0 / 16), reads=["A2"], writes=["E1"])
        p.op("dve", lambda e: e.tensor_tensor(out=kf[:], in0=kT[:], in1=E1[:], op=ALU.mult), reads=["kT", "E1"], writes=["kf"])
        if not ctx:
            p.op("act", lambda e: e.activation(out=E1[:], in_=A2[:], func=AF.Exp, scale=-1.0 / 16), reads=["A2"], writes=["E1"])
            p.op("dve", lambda e: e.tensor_tensor(out=qf[:], in0=qT[:], in1=E1[:], op=ALU.mult), reads=["qT", "E1"], writes=["qf"])
        softplus_scan(h, 1, A1, A2)
        p.op("dve", lambda e: e.tensor_tensor(out=A2c, in0=A2c[:, :, 127:128].to_broadcast([128, NCH, 128]), in1=A2c, op=ALU.subtract),
             reads=["A2"], writes=["A2"])
        if not ctx:
            p.op("act", lambda e: e.activation(out=E1[:], in_=A2[:], func=AF.Exp, scale=-1.0 / 16), reads=["A2"], writes=["E1"])
            p.op("dve", lambda e: e.tensor_tensor(out=qb[:], in0=qT[:], in1=E1[:], op=ALU.mult), reads=["qT", "E1"], writes=["qb"])
        p.op("dve", lambda e: e.tensor_tensor(out=A1[:], in0=A1[:], in1=A2[:], op=ALU.add), reads=["A1", "A2"], writes=["A1"])
        A1c = A1[:].rearrange("p (c t) -> p c t", t=128)
        p.op("act", lambda e: e.activation(out=Db[:], in_=A1c[:, :, 0], func=AF.Exp, scale=-1.0 / 16), reads=["A1"], writes=["Db"])
        p.op("act", lambda e: e.activation(out=E1[:], in_=A1[:], func=AF.Exp, scale=1.0 / 16), reads=["A1"], writes=["E1"])
        p.op("dve", lambda e: e.tensor_tensor(out=kb[:], in0=kT[:], in1=E1[:], op=ALU.mult), reads=["kT", "E1"], writes=["kb"])

        def make_ktok(ktil, dst, dkey, skey):
            for c4 in range(NCH // 4 if NCH >= 4 else 1):
                n4 = min(4, NCH)
                bank = 2 + c4 % 2
                pv = g.ps[bank][:].bitcast(BF16)
                for j in range(n4):
                    c = c4 * 4 + j
                    p.op("pe", lambda e, c=c, j=j, pv=pv: e.transpose(pv[:, j * 128:(j + 1) * 128], ktil[:, c * 128:(c + 1) * 128], g.ident_b[:]),
                         reads=[skey, "ident_b"], writes=["ps%d" % bank])
                dv = dst[:, c4 * 4:c4 * 4 + n4, :].rearrange("p a b -> p (a b)")
                if c4 % 2 == 0:
                    p.op("act", lambda e, dv=dv, pv=pv, n4=n4: e.activation(out=dv, in_=pv[:, 0:n4 * 128], func=AF.Copy), reads=["ps%d" % bank], writes=[dkey])
                else:
                    p.op("dve", lambda e, dv=dv, pv=pv, n4=n4: e.tensor_copy(out=dv, in_=pv[:, 0:n4 * 128]), reads=["ps%d" % bank], writes=[dkey])

        make_ktok(kb, ktk_b, "ktk_b", "kb")
        make_ktok(kf, ktk_f, "ktk_f", "kf")

        def state_step(ktk, kkey, c, Dv, store_bf=None, n=[0]):
            n[0] += 1
            bank = 2 + n[0] % 2
            pd = g.ps[bank]
            pk = "ps%d" % bank
            p.op("pe", lambda e: e.matmul(pd[:, 0:256], lhsT=ktk[:, c, :], rhs=vh[:, c, :], start=True, stop=True), reads=[kkey, "vh"], writes=[pk])
            p.op("dve", lambda e: e.tensor_tensor(out=Stmp[:], in0=pd[:, 0:256], in1=S32[:], op=ALU.add), reads=[pk, "S32"], writes=["Stmp"])
            p.op("act", lambda e: e.activation(out=S32[:], in_=Stmp[:], func=AF.Copy, scale=Dv[:, c:c + 1]), reads=["Stmp", "Dq", "Db"], writes=["S32"])
            if store_bf is not None:
                dst, key = store_bf
                p.op("act", lambda e: e.activation(out=dst, in_=Stmp[:], func=AF.Copy, scale=Dv[:, c:c + 1]),
                     reads=["Stmp", "Dq", "Db"], writes=[key])

        if ctx:
            p.op("dve", lambda e: e.memset(S32[:], 0.0), writes=["S32"])
        else:
            p.op("dve", lambda e: e.tensor_copy(out=S32[:], in_=g.s0[:, b, 1, h, :]), reads=["s0"], writes=["S32"])
            p.op("act", lambda e: e.activation(out=Rbf[:, NCH - 1, :], in_=g.s0[:, b, 1, h, :], func=AF.Copy), reads=["s0"], writes=["Rbf"])
        for c in range(NCH - 1, -1, -1):
            if ctx or c == 0:
                state_step(ktk_b, "ktk_b", c, Db)
            else:
                state_step(ktk_b, "ktk_b", c, Db, store_bf=(Rbf[:, c - 1, :], "Rbf"))
        if ctx:
            p.op("dve", lambda e: e.tensor_copy(out=g.s0[:, b, 1, h, :], in_=S32[:]), reads=["S32"], writes=["s0"])
        if ctx:
            p.op("dve", lambda e: e.memset(S32[:], 0.0), writes=["S32"])
            for c in range(NCH):
                state_step(ktk_f, "ktk_f", c, Dq)
            p.op("dve", lambda e: e.tensor_copy(out=g.s0[:, b, 0, h, :], in_=S32[:]), reads=["S32"], writes=["s0"])
            return
        p.op("dve", lambda e: e.tensor_copy(out=S32[:], in_=g.s0[:, b, 0, h, :]), reads=["s0"], writes=["S32"])
        p.op("act", lambda e: e.activation(out=Sbf[0][:], in_=g.s0[:, b, 0, h, :], func=AF.Copy), reads=["s0"], writes=["Sbf0"])

        def stA(c):
            ts = slice(c * 128, (c + 1) * 128)
            Ac, Ak = Acm[c % 2], "Acm%d" % (c % 2)
            p.op("pe", lambda e: e.matmul(g.ps[4][:, 0:128], lhsT=kf[:, ts], rhs=qf[:, ts], start=True, stop=True), reads=["kf", "qf"], writes=["ps4"])
            p.op("pe", lambda e: e.matmul(g.ps[5][:, 0:128], lhsT=kb[:, ts], rhs=qb[:, ts], start=True, stop=True), reads=["kb", "qb"], writes=["ps5"])
            p.op("dve", lambda e: e.tensor_tensor(out=Atmp[:], in0=g.ps[4][:, 0:128], in1=mkf[:], op=ALU.mult), reads=["ps4", "mkf"], writes=["Atmp"])
            p.op("dve", lambda e: e.tensor_tensor(out=Atm2[:], in0=g.ps[5][:, 0:128], in1=mkb[:], op=ALU.mult), reads=["ps5", "mkb"], writes=["Atm2"])
            p.op("dve", lambda e: e.tensor_tensor(out=Ac[:], in0=Atmp[:], in1=Atm2[:], op=ALU.add), reads=["Atmp", "Atm2"], writes=[Ak])

        def stO1(c):
            ts = slice(c * 128, (c + 1) * 128)
            Sb, Sk = Sbf[c % 2], "Sbf%d" % (c % 2)
            Ac, Ak = Acm[c % 2], "Acm%d" % (c % 2)
            po, pok = g.ps[6 + c % 2], "ps%d" % (6 + c % 2)
            for half in range(2):
                hs = slice(half * 128, (half + 1) * 128)
                p.op("pe", lambda e, half=half, hs=hs: e.matmul(po[:, half * 128:(half + 1) * 128], lhsT=vh[:, c, hs], rhs=Ac[:], start=True, stop=False),
                     reads=["vh", Ak], writes=[pok])
                p.op("pe", lambda e, half=half, hs=hs: e.matmul(po[:, half * 128:(half + 1) * 128], lhsT=Sb[:, hs], rhs=qf[:, ts], start=False, stop=False),
                     reads=[Sk, "qf"], writes=[pok])
                p.op("pe", lambda e, half=half, hs=hs: e.matmul(po[:, half * 128:(half + 1) * 128], lhsT=Rbf[:, c, hs], rhs=qb[:, ts], start=False, stop=True),
                     reads=["Rbf", "qb"], writes=[pok])
            sq_ = sq2[c % 2]
            p.op("act", lambda e: e.activation(out=sq_[:].rearrange("p a t -> p (a t)"), in_=po[:, 0:256], func=AF.Square), reads=[pok], writes=["sq%d" % (c % 2)])

        def stO2(c):
            ts = slice(c * 128, (c + 1) * 128)
            po, pok = g.ps[6 + c % 2], "ps%d" % (6 + c % 2)
            sq_ = sq2[c % 2]
            pss = g.ps[2 + c % 2]
            psk = "ps%d" % (2 + c % 2)
            for half in range(2):
                p.op("pe", lambda e, half=half: e.matmul(pss[:, 256:384], lhsT=ones_b[:], rhs=sq_[:, half, :], start=(half == 0), stop=(half == 1)),
                     reads=["ones_b", "sq%d" % (c % 2)], writes=[psk])
            p.op("act", lambda e: e.activation(out=rs[:], in_=pss[:, 256:384], func=AF.Ln, scale=1.0 / 256, bias=EPS), reads=[psk], writes=["rs"])
            p.op("act", lambda e: e.activation(out=rs[:], in_=rs[:], func=AF.Exp, scale=-0.5), reads=["rs"], writes=["rs"])
            for half in range(2):
                p.op("dve", lambda e, half=half: e.tensor_tensor(out=ytmp[:, half, :], in0=po[:, half * 128:(half + 1) * 128], in1=rs[:], op=ALU.mult),
                     reads=[pok, "rs"], writes=["ytmp"])
                p.op("dve", lambda e, half=half: e.scalar_tensor_tensor(out=sg[:, half, ts], in0=ytmp[:, half, :], scalar=gnw[:, h, half:half + 1],
                                                                      in1=sg[:, half, ts], op0=ALU.mult, op1=ALU.mult),
                     reads=["ytmp", "gnw", "sg"], writes=["sg"])

        def stS(c):
            Sn, Snk = Sbf[(c + 1) % 2], "Sbf%d" % ((c + 1) % 2)
            state_step(ktk_f, "ktk_f", c, Dq, store_bf=(Sn[:], Snk))

        stA(0)
        for c in range(NCH):
            stO1(c)
            if c + 1 < NCH:
                stA(c + 1)
            if c >= 1:
                stO2(c - 1)
            if c + 1 < NCH:
                stS(c)
        stO2(NCH - 1)
        p.dma("act", g.yglaT[b][h * 256:(h + 1) * 256, :].rearrange("(a p) t -> p a t", p=128), sg[:], reads=["sg"], writes=["scr"])

    for h in range(4):
        do_head(h)
    p.pop()


def phase_filter(g):
    p = g.p
    p.push()
    PI = math.pi
    zx = p.sbuf("zx", [33, 4096], F32)
    h1 = p.sbuf("h1", [64, 4096], F32)
    mt = p.sbuf("mt", [64, 4096], F32)
    h2b = p.sbuf("h2b", [64, 8192], BF16)
    w1 = p.sbuf("w1", [33, 64], F32)
    w2 = p.sbuf("w2", [64, 64], F32)
    w3s = p.sbuf("w3s", [64, 512], F32)
    w3b = p.sbuf("w3b", [64, 4096], BF16)
    bf = p.sbuf("bf", [64, 4], F32)
    fb = p.sbuf("fb", [64, 2], F32)
    dl = p.sbuf("dl", [128, 8], F32)
    ndl = p.sbuf("ndl", [128, 8], F32)
    tx = p.sbuf("tx", [128, 8192], F32)
    dec = p.sbuf("dec", [128, 4096], F32)
    kern = p.sbuf("kern", [128, 8192], F32)
    kbf = p.sbuf("kbf", [128, 8192], BF16)
    st = p.sbuf("st", [128, 4], F32)
    p.dma("sp", w1[:], g.hy_w1, writes=["w1"])
    p.dma("sp", w2[:], g.hy_w2, writes=["w2"])
    p.dma("sp", bf[:], g.hy_bf, writes=["bf"])
    p.dma("sp", dl[:], g.deltaT, writes=["dl"])
    p.dma("sp", tx[:], g.text[0:1, :].partition_broadcast(128), writes=["tx"])
    for i in range(8):
        p.dma("sp", w3s[:], g.hy_w3[:, i * 512:(i + 1) * 512], writes=["w3s"])
        p.op("dve", lambda e, i=i: e.tensor_copy(out=w3b[:, i * 512:(i + 1) * 512], in_=w3s[:]), reads=["w3s"], writes=["w3b"])
    p.op("dve", lambda e: e.tensor_tensor(out=fb[:], in0=bf[:, 0:2], in1=bf[:, 2:4], op=ALU.mult), reads=["bf"], writes=["fb"])
    p.op("dve", lambda e: e.tensor_scalar(out=ndl[:], in0=dl[:], scalar1=-1.0, scalar2=None, op0=ALU.mult), reads=["dl"], writes=["ndl"])

    def sin_layer(src, K, wt, li, dst_f32=None, dst_bf=None):
        for ch in range(8):
            cs = slice(ch * 512, (ch + 1) * 512)
            ps = g.ps[ch % 2]
            pk = "ps%d" % (ch % 2)
            p.op("pe", lambda e, ps=ps, cs=cs: e.matmul(ps[0:64, :], lhsT=wt[0:K, :], rhs=src[0:K, cs], start=True, stop=True),
                 reads=["w1", "w2", "zx", "h1"], writes=[pk])
            p.op("act", lambda e, ps=ps, cs=cs: e.activation(out=mt[:, cs], in_=ps[0:64, :], func=AF.Identity, scale=bf[:, 2 + li:3 + li], bias=fb[:, li:li + 1]),
                 reads=[pk, "bf", "fb"], writes=["mt"])
        tmp = kern[0:64, 0:4096]
        for rep in range(2):
            p.op("dve", lambda e: e.tensor_scalar(out=tmp, in0=mt[:], scalar1=PI, scalar2=-2 * PI, op0=ALU.is_gt, op1=ALU.mult), reads=["mt"], writes=["kern"])
            p.op("dve", lambda e: e.tensor_tensor(out=mt[:], in0=mt[:], in1=tmp, op=ALU.add), reads=["mt", "kern"], writes=["mt"])
            p.op("dve", lambda e: e.tensor_scalar(out=tmp, in0=mt[:], scalar1=-PI, scalar2=2 * PI, op0=ALU.is_lt, op1=ALU.mult), reads=["mt"], writes=["kern"])
            p.op("dve", lambda e: e.tensor_tensor(out=mt[:], in0=mt[:], in1=tmp, op=ALU.add), reads=["mt", "kern"], writes=["mt"])
        p.op("dve", lambda e: e.tensor_scalar(out=mt[:], in0=mt[:], scalar1=PI, scalar2=-PI, op0=ALU.min, op1=ALU.max), reads=["mt"], writes=["mt"])
        if dst_f32 is not None:
            p.op("act", lambda e: e.activation(out=dst_f32, in_=mt[:], func=AF.Sin), reads=["mt"], writes=["h1"])
        else:
            p.op("act", lambda e: e.activation(out=dst_bf, in_=mt[:], func=AF.Sin), reads=["mt"], writes=["h2b"])

    for nh in range(2):
        p.dma("sp", zx[:], g.zext[:, nh * 4096:(nh + 1) * 4096], writes=["zx"])
        sin_layer(zx, 33, w1, 0, dst_f32=h1[:])
        sin_layer(h1, 64, w2, 1, dst_bf=h2b[:, nh * 4096:(nh + 1) * 4096])

    def one(o, cc):
        for nh in range(2):
            od = 2 * o + nh
            p.op("act", lambda e, nh=nh: e.activation(out=dec[:], in_=tx[:, nh * 4096:(nh + 1) * 4096], func=AF.Exp, scale=ndl[:, cc:cc + 1]),
                 reads=["tx", "ndl"], writes=["dec"])
            for ch in range(8):
                ps = g.ps[2 + ch % 4]
                pk = "ps%d" % (2 + ch % 4)
                cs = slice(nh * 4096 + ch * 512, nh * 4096 + (ch + 1) * 512)
                p.op("pe", lambda e, ps=ps, cs=cs, od=od: e.matmul(ps[:], lhsT=w3b[:, od * 1024 + cc * 128:od * 1024 + (cc + 1) * 128], rhs=h2b[:, cs], start=True, stop=True),
                     reads=["w3b", "h2b"], writes=[pk])
                p.op("dve", lambda e, ps=ps, cs=cs, ch=ch: e.tensor_tensor(out=kern[:, cs], in0=ps[:], in1=dec[:, ch * 512:(ch + 1) * 512], op=ALU.mult),
                     reads=[pk, "dec"], writes=["kern"])
            p.op("act", lambda e, nh=nh: e.activation(out=dec[:], in_=kern[:, nh * 4096:(nh + 1) * 4096], func=AF.Square, accum_out=st[:, nh:nh + 1]),
                 reads=["kern"], writes=["dec", "st"])
        p.op("dve", lambda e: e.tensor_tensor(out=st[:, 2:3], in0=st[:, 0:1], in1=st[:, 1:2], op=ALU.add), reads=["st"], writes=["st"])
        p.op("act", lambda e: e.activation(out=st[:, 3:4], in_=st[:, 2:3], func=AF.Ln), reads=["st"], writes=["st"])
        p.op("act", lambda e: e.activation(out=st[:, 3:4], in_=st[:, 3:4], func=AF.Exp, scale=-0.5), reads=["st"], writes=["st"])
        p.op("dve", lambda e: e.tensor_scalar(out=kbf[:], in0=kern[:], scalar1=st[:, 3:4], scalar2=None, op0=ALU.mult), reads=["kern", "st"], writes=["kbf"])
        p.dma("act", g.kernT[o][cc * 128:(cc + 1) * 128, :], kbf[:], reads=["kbf"], writes=["scr"])

    p.dma("sp", g.dbg_h2, h2b[:], reads=["h2b"], writes=["dbgh2"])
    p.dma("sp", g.dbg_h1, h1[:], reads=["h1"], writes=["dbgh1"])
    p.dma("sp", g.dbg_mt, mt[:], reads=["mt"], writes=["dbgmt"])
    for o in range(2):
        for cc in range(8):
            one(o, cc)
            if o == 0 and cc == 0:
                p.dma("sp", g.dbg_kern, kern[:], reads=["kern"], writes=["dbgk"])
                p.dma("sp", g.dbg_st, st[:], reads=["st"], writes=["dbgst"])
    p.pop()


CG = 64


def phase_hyena(g, dbg):
    p = g.p
    p.push()
    Gt = p.sbuf("Gt", [128, 64, 2, 128], BF16)
    Gp = p.sbuf("Gp", [128, 128, 32], BF16)
    F64t = p.sbuf("F64t", [64, 192], BF16)
    C3 = p.sbuf("C3", [128, 3, 128], BF16)
    hbT = p.sbuf("hbT", [64, 16, 2], F32)
    Xs = p.sbuf("Xs", [64, CG, 128], BF16)
    Asb = p.sbuf("Asb", [128, 3, 64, CG], BF16)
    Ssb = p.sbuf("Ssb", [128, 64, 2, CG], BF16)
    V = p.sbuf("V", [128, 2, 64, CG], BF16)
    Ub = [p.sbuf("Ub%d" % i, [128, 4, 2, CG], BF16) for i in range(4)]
    tv = [p.sbuf("tv%d" % i, [128, 2, 4, 2, CG], BF16) for i in range(4)]
    Psb = p.sbuf("Psb", [128, CG, 128], BF16)
    zin = p.sbuf("zin", [CG, L], BF16)
    prt = p.sbuf("prt", [CG, L], BF16)
    zout = p.sbuf("zout", [CG, L], BF16)
    ty = [p.sbuf("ty%d" % i, [CG, 16, 32], F32) for i in range(2)]
    p.push()
    stg = p.sbuf("stg", [128, 8, 2, 128], F32)
    p.dma("sp", hbT[:], g.hy_biasT, writes=["hbT"])
    for i in range(8):
        p.dma("sp", stg[:], g.tG[:, i * 8:(i + 1) * 8, :, :], writes=["stg"])
        p.op("dve", lambda e, i=i: e.tensor_copy(out=Gt[:, i * 8:(i + 1) * 8, :, :], in_=stg[:]), reads=["stg"], writes=["Gt"])
    stg2 = stg[:].rearrange("p a b c -> p (a b c)")
    for i in range(2):
        p.dma("sp", stg2[:, 0:2048], g.tGp[:, i * 64:(i + 1) * 64, :].rearrange("p a b -> p (a b)"), writes=["stg"])
        p.op("dve", lambda e, i=i: e.tensor_copy(out=Gp[:, i * 64:(i + 1) * 64, :].rearrange("p a b -> p (a b)"), in_=stg2[:, 0:2048]), reads=["stg"], writes=["Gp"])
    p.dma("sp", stg2[0:64, 0:192], g.tF64, writes=["stg"])
    p.op("dve", lambda e: e.tensor_copy(out=F64t[:], in_=stg2[0:64, 0:192]), reads=["stg"], writes=["F64t"])
    p.dma("sp", stg2[:, 0:384], g.tC3.rearrange("p a b -> p (a b)"), writes=["stg"])
    p.op("dve", lambda e: e.tensor_copy(out=C3[:].rearrange("p a b -> p (a b)"), in_=stg2[:, 0:384]), reads=["stg"], writes=["C3"])
    p.pop()

    cnt = [0]

    def fwd_stage1(src_rows, K):
        if src_rows is not None:
            p.dma("sp", Xs[0:K, :, :], src_rows.rearrange("c (n1 n2) -> n1 c n2", n2=128), writes=["Xs"])
        for c2 in range(CG // 2):
            bank = c2 % 2
            ps = g.ps[bank]
            pk = "ps%d" % bank
            for j in range(2):
                c = c2 * 2 + j
                p.op("pe", lambda e, ps=ps, c=c, j=j: e.matmul(ps[:, j * 192:(j + 1) * 192], lhsT=Xs[0:K, c, :], rhs=F64t[0:K, :], start=True, stop=True),
                     reads=["Xs", "F64t"], writes=[pk])
            src = ps[:, 0:384].rearrange("p (c m k) -> p m k c", c=2, m=3)
            dst = Asb[:, :, :, c2 * 2:c2 * 2 + 2]
            if c2 % 2 == 0:
                p.op("act", lambda e, src=src, dst=dst: e.activation(out=dst, in_=src, func=AF.Copy), reads=[pk], writes=["Asb"])
            else:
                p.op("dve", lambda e, src=src, dst=dst: e.tensor_copy(out=dst, in_=src), reads=[pk], writes=["Asb"])

    def fwd_stage2(k1, bank, q):
        ps = g.ps[bank]
        pk = "ps%d" % bank
        ure = ps[:, q * 128:q * 128 + CG]
        uim = ps[:, q * 128 + CG:q * 128 + 2 * CG]
        p.op("pe", lambda e: e.matmul(ure, lhsT=Gt[:, k1, 0, :], rhs=Asb[:, 0, k1, :], start=True, stop=False), reads=["Gt", "Asb"], writes=[pk])
        p.op("pe", lambda e: e.matmul(ure, lhsT=Gt[:, k1, 1, :], rhs=Asb[:, 2, k1, :], start=False, stop=True), reads=["Gt", "Asb"], writes=[pk])
        p.op("pe", lambda e: e.matmul(uim, lhsT=Gt[:, k1, 1, :], rhs=Asb[:, 0, k1, :], start=True, stop=False), reads=["Gt", "Asb"], writes=[pk])
        p.op("pe", lambda e: e.matmul(uim, lhsT=Gt[:, k1, 0, :], rhs=Asb[:, 1, k1, :], start=False, stop=True), reads=["Gt", "Asb"], writes=[pk])

    def fwd_stage2_block(kb):
        cnt[0] += 1
        bank = (2, 3, 0, 1)[cnt[0] % 4]
        for q in range(4):
            fwd_stage2(kb * 4 + q, bank, q)
        return g.ps[bank][:].rearrange("p (k m c) -> p k m c", k=4, m=2), "ps%d" % bank

    for o in range(2):
        for cgi in range(16):
            fwd_stage1(g.kernT[o][cgi * CG:(cgi + 1) * CG, :], 64)
            for kb in range(16):
                u, pk = fwd_stage2_block(kb)
                if kb % 2 == 0:
                    p.op("act", lambda e, u=u, kb=kb: e.activation(out=Ssb[:, kb * 4:(kb + 1) * 4, :, :], in_=u, func=AF.Copy), reads=[pk], writes=["Ssb"])
                else:
                    p.op("dve", lambda e, u=u, kb=kb: e.tensor_copy(out=Ssb[:, kb * 4:(kb + 1) * 4, :, :], in_=u), reads=[pk], writes=["Ssb"])
            p.dma("act", g.sspec[o][cgi], Ssb[:].rearrange("p a b c -> p (a b c)"), reads=["Ssb"], writes=["sspec"])
    if "stop6" in dbg:
        p.pop()
        return

    zin2 = [zin, p.sbuf("zin_b", [CG, L], BF16)]
    prt2 = [prt, p.sbuf("prt_b", [CG, L], BF16)]
    units = [(b, o, cgi) for b in range(NB) for o in range(2) for cgi in range(16)]
    if "stop7" in dbg:
        units = units[:16]

    def srcdst(b, o):
        return (g.uhy[b][0] if o == 0 else g.z1T[b]), (g.z1T[b] if o == 0 else g.yhyT[b])

    def stA(i):
        b, o, cgi = units[i]
        rows = slice(cgi * CG, (cgi + 1) * CG)
        zsrc, _ = srcdst(b, o)
        zk = [("z", b, o - 1, cgi)] if o == 1 else []
        p.dma("sp", Xs[0:32, :, :], zsrc[rows, :].rearrange("c (n1 n2) -> n1 c n2", n2=128), reads=zk, writes=["Xs"])
        p.dma("sp", Ssb[:].rearrange("p a b c -> p (a b c)"), g.sspec[o][cgi], writes=["Ssb"])
        p.dma("sp", zin2[i % 2][:], zsrc[rows, :], reads=zk, writes=["zin%d" % (i % 2)])
        p.dma("sp", prt2[i % 2][:], g.uhy[b][o + 1][rows, :], writes=["prt%d" % (i % 2)])
        fwd_stage1(None, 32)

    def stB(i):
        for kb in range(16):
            u, pk = fwd_stage2_block(kb)
            ub, uk = Ub[kb % 4], "Ub%d" % (kb % 4)
            t, tk = tv[kb % 4], "tv%d" % (kb % 4)
            ks = slice(kb * 4, (kb + 1) * 4)
            p.op("act", lambda e, u=u, ub=ub: e.activation(out=ub[:], in_=u, func=AF.Copy), reads=[pk], writes=[uk])
            p.op("dve", lambda e, ub=ub, t=t, ks=ks: e.tensor_tensor(out=t[:, 0], in0=ub[:], in1=Ssb[:, ks, 0:1, :].to_broadcast([128, 4, 2, CG]), op=ALU.mult),
                 reads=[uk, "Ssb"], writes=[tk])
            p.op("dve", lambda e, ub=ub, t=t, ks=ks: e.tensor_tensor(out=t[:, 1], in0=ub[:], in1=Ssb[:, ks, 1:2, :].to_broadcast([128, 4, 2, CG]), op=ALU.mult),
                 reads=[uk, "Ssb"], writes=[tk])
            p.op("pool", lambda e, t=t, ks=ks: e.tensor_tensor(out=V[:, 0, ks, :], in0=t[:, 0, :, 0, :], in1=t[:, 1, :, 1, :], op=ALU.subtract), reads=[tk], writes=["V"])
            p.op("pool", lambda e, t=t, ks=ks: e.tensor_tensor(out=V[:, 1, ks, :], in0=t[:, 1, :, 0, :], in1=t[:, 0, :, 1, :], op=ALU.add), reads=[tk], writes=["V"])

    def stC(i):
        for c4 in range(CG // 4):
            bank = 4 + c4 % 2
            ps = g.ps[bank]
            pk = "ps%d" % bank
            for j in range(4):
                c = c4 * 4 + j
                cs = slice(j * 128, (j + 1) * 128)
                p.op("pe", lambda e, ps=ps, c=c, cs=cs: e.matmul(ps[0:64, cs], lhsT=V[:, 0, :, c], rhs=C3[:, 0, :], start=True, stop=False), reads=["V", "C3"], writes=[pk])
                p.op("pe", lambda e, ps=ps, c=c, cs=cs: e.matmul(ps[0:64, cs], lhsT=V[:, 1, :, c], rhs=C3[:, 2, :], start=False, stop=True), reads=["V", "C3"], writes=[pk])
                p.op("pe", lambda e, ps=ps, c=c, cs=cs: e.matmul(ps[64:128, cs], lhsT=V[:, 0, :, c], rhs=C3[:, 1, :], start=True, stop=False), reads=["V", "C3"], writes=[pk])
                p.op("pe", lambda e, ps=ps, c=c, cs=cs: e.matmul(ps[64:128, cs], lhsT=V[:, 1, :, c], rhs=C3[:, 0, :], start=False, stop=True), reads=["V", "C3"], writes=[pk])
            dst = Psb[:, c4 * 4:(c4 + 1) * 4, :].rearrange("p a b -> p (a b)")
            if c4 % 2 == 0:
                p.op("act", lambda e, ps=ps, dst=dst: e.activation(out=dst, in_=ps[:], func=AF.Copy), reads=[pk], writes=["Psb"])
            else:
                p.op("dve", lambda e, ps=ps, dst=dst: e.tensor_copy(out=dst, in_=ps[:]), reads=[pk], writes=["Psb"])

    def stD(i):
        b, o, cgi = units[i]
        rows = slice(cgi * CG, (cgi + 1) * CG)
        _, zdst = srcdst(b, o)
        zi, pr_ = zin2[i % 2], prt2[i % 2]
        zin_v = zi[:].rearrange("c (tb ta) -> c ta tb", ta=128)
        prt_v = pr_[:].rearrange("c (tb ta) -> c ta tb", ta=128)
        zout_v = zout[:].rearrange("c (tb ta) -> c ta tb", ta=128)
        for r in range(8):
            bank = 6 + r % 2
            ps = g.ps[bank]
            pk = "ps%d" % bank
            for j in range(16):
                ta = r * 16 + j
                p.op("pe", lambda e, ps=ps, ta=ta, j=j: e.matmul(ps[0:CG, j * 32:(j + 1) * 32], lhsT=Psb[:, :, ta], rhs=Gp[:, ta, :], start=True, stop=True),
                     reads=["Psb", "Gp"], writes=[pk])
            t = ty[r % 2]
            tk = "ty%d" % (r % 2)
            tas = slice(r * 16, (r + 1) * 16)
            p.op("dve", lambda e, ps=ps, t=t, tas=tas: e.scalar_tensor_tensor(out=t[:], in0=zin_v[:, tas, :], scalar=hbT[:, cgi, o:o + 1],
                                                                            in1=ps[0:CG, :].rearrange("c (a b) -> c a b", b=32), op0=ALU.mult, op1=ALU.add),
                 reads=[pk, "zin%d" % (i % 2), "hbT"], writes=[tk])
            p.op("pool", lambda e, t=t, tas=tas: e.tensor_tensor(out=zout_v[:, tas, :], in0=t[:], in1=prt_v[:, tas, :], op=ALU.mult),
                 reads=[tk, "prt%d" % (i % 2)], writes=["zout"])
        p.dma("act", zdst[rows, :], zout[:], reads=["zout"], writes=[("z", b, o, cgi)])

    stA(0)
    for i in range(len(units)):
        stB(i)
        if i + 1 < len(units):
            stA(i + 1)
        stC(i)
        stD(i)
    p.pop()


def phase_merge(g, b, dbg=()):
    p = g.p
    p.push()
    wst = p.sbuf("wst", [128, 8, 512], F32)
    phb = p.sbuf("phb", [128, 8, D], BF16)
    pgb = p.sbuf("pgb", [128, 8, D], BF16)
    wob = p.sbuf("wob", [128, 8, D], BF16)
    g1b = p.sbuf("g1b", [128, D], F32)
    rw = p.sbuf("rw", [128, 8, 64], F32)
    rb = p.sbuf("rb", [128, 64], F32)
    yh = p.sbuf("yh", [128, 8, 512], BF16)
    yg = p.sbuf("yg", [128, 8, 512], BF16)
    gt = p.sbuf("gt", [128, 16, 512], BF16)
    mg = p.sbuf("mg", [128, 8, 512], BF16)
    t1 = p.sbuf("t1", [128, 512], F32)
    t2 = p.sbuf("t2", [128, 512], F32)
    xt = [p.sbuf("xt%d" % i, [128, D], F32) for i in range(2)]
    x1 = [p.sbuf("x1_%d" % i, [128, D], F32) for i in range(2)]
    xs = p.sbuf("xs", [128, D], F32)
    junk = p.sbuf("junk", [128, D], F32)
    stat = p.sbuf("stat", [128, 8], F32)
    xnf = p.sbuf("xnf", [128, 8, 128], F32)
    xnb = [p.sbuf("xnb%d" % i, [128, 8, 128], BF16) for i in range(2)]
    sc = p.sbuf("sc", [128, 64], F32)
    sl = p.sbuf("sl", [128, 64], F32)
    sl2 = p.sbuf("sl2", [128, 64], F32)
    eq = p.sbuf("eq", [128, 64], F32)
    gs = p.sbuf("gs", [128, 8], F32)
    m1 = p.sbuf("m1", [128, 8], F32)
    m2 = p.sbuf("m2", [128, 8], F32)
    gm = p.sbuf("gm", [128, 8], F32)
    mx = p.sbuf("mx", [128, 8], F32)
    cmb = p.sbuf("cmb", [128, 64], F32)
    cmT = [p.sbuf("cmT%d" % i, [64, 128], BF16) for i in range(2)]
    p.dma("sp", g1b[:], g.gbc_d[:, b * D:(b + 1) * D], writes=["g1b"])
    p.dma("sp", rw[:], g.router_w.rearrange("(k p) n -> p k n", p=128), writes=["rw"])
    p.dma("sp", rb[:], g.router_b[0:1, :].partition_broadcast(128), writes=["rb"])
    for wi, (src, dst, key) in enumerate([(g.proj_hy, phb, "phb"), (g.proj_gla, pgb, "pgb"), (g.w_out, wob, "wob")]):
        sv = src.rearrange("(k p) n -> p k n", p=128)
        for hf in range(2):
            cs = slice(hf * 512, (hf + 1) * 512)
            p.dma("sp", wst[:], sv[:, :, cs], writes=["wst"])
            if wi < 2:
                p.op("pool", lambda e, dst=dst, cs=cs: e.tensor_copy(out=dst[:, :, cs], in_=wst[:]), reads=["wst"], writes=[key])
            else:
                p.op("dve", lambda e, dst=dst, cs=cs: e.tensor_tensor(out=dst[:, :, cs], in0=wst[:], in1=g1b[:, cs].unsqueeze(1).to_broadcast([128, 8, 512]), op=ALU.mult),
                     reads=["wst", "g1b"], writes=[key])
    sc2 = [sc, p.sbuf("sc_b", [128, 64], F32)]
    NTILE = L // 128

    def stG(tg):
        ts = slice(tg * 512, (tg + 1) * 512)
        p.dma("sp", yh[:], g.yhyT[b].rearrange("(k p) t -> p k t", p=128)[:, :, ts], writes=["yh"])
        p.dma("sp", yg[:], g.yglaT[b].rearrange("(k p) t -> p k t", p=128)[:, :, ts], writes=["yg"])
        p.dma("sp", gt[:], g.gate[b].rearrange("(k p) t -> p k t", p=128)[:, :, ts], writes=["gt"])
        for dc in range(8):
            ds_ = slice(dc * 128, (dc + 1) * 128)
            pa, pb = g.ps[dc % 2], g.ps[2]
            ka = "ps%d" % (dc % 2)
            for k in range(8):
                p.op("pe", lambda e, pa=pa, k=k, ds_=ds_: e.matmul(pa[:], lhsT=phb[:, k, ds_], rhs=yh[:, k, :], start=(k == 0), stop=(k == 7)), reads=["phb", "yh"], writes=[ka])
            for k in range(8):
                p.op("pe", lambda e, pb=pb, k=k, ds_=ds_: e.matmul(pb[:], lhsT=pgb[:, k, ds_], rhs=yg[:, k, :], start=(k == 0), stop=(k == 7)), reads=["pgb", "yg"], writes=["ps2"])
            p.op("dve", lambda e, pa=pa, dc=dc: e.tensor_tensor(out=t1[:], in0=pa[:], in1=gt[:, dc, :], op=ALU.mult), reads=[ka, "gt"], writes=["t1"])
            p.op("dve", lambda e, pb=pb, dc=dc: e.tensor_tensor(out=t2[:], in0=pb[:], in1=gt[:, 8 + dc, :], op=ALU.mult), reads=["ps2", "gt"], writes=["t2"])
            p.op("pool", lambda e, dc=dc: e.tensor_tensor(out=mg[:, dc, :], in0=t1[:], in1=t2[:], op=ALU.add), reads=["t1", "t2"], writes=["mg"])

    def stY(t):
        i = t % 2
        tt = t % 4
        p.dma("sp", xt[i][:], g.x[b][t * 128:(t + 1) * 128, :], writes=["xt%d" % i])
        for hf in range(2):
            for k in range(8):
                p.op("pe", lambda e, hf=hf, k=k: e.matmul(g.ps[4 + hf][:], lhsT=mg[:, k, tt * 128:(tt + 1) * 128], rhs=wob[:, k, hf * 512:(hf + 1) * 512],
                                                          start=(k == 0), stop=(k == 7)), reads=["mg", "wob"], writes=["ps%d" % (4 + hf)])

    def stT1(t):
        i = t % 2
        xt_, x1_ = xt[i], x1[i]
        tsl = slice(t * 128, (t + 1) * 128)
        for hf in range(2):
            cs = slice(hf * 512, (hf + 1) * 512)
            p.op("dve", lambda e, hf=hf, cs=cs: e.tensor_tensor(out=x1_[:, cs], in0=g.ps[4 + hf][:], in1=xt_[:, cs], op=ALU.add),
                 reads=["ps%d" % (4 + hf), "xt%d" % i], writes=["x1_%d" % i])
        p.dma("act", g.x1d[b][tsl, :], x1_[:], reads=["x1_%d" % i], writes=["scr"])
        p.op("act", lambda e: e.activation(out=junk[:], in_=x1_[:], func=AF.Square, accum_out=stat[:, 0:1]), reads=["x1_%d" % i], writes=["junk", "stat"])
        p.op("act", lambda e: e.activation(out=stat[:, 1:2], in_=stat[:, 0:1], func=AF.Ln, scale=1.0 / D, bias=EPS), reads=["stat"], writes=["stat"])
        p.op("act", lambda e: e.activation(out=stat[:, 2:3], in_=stat[:, 1:2], func=AF.Exp, scale=-0.5), reads=["stat"], writes=["stat"])
        p.op("dve", lambda e: e.tensor_scalar(out=xs[:], in0=x1_[:], scalar1=stat[:, 2:3], scalar2=None, op0=ALU.mult), reads=["x1_%d" % i, "stat"], writes=["xs"])

    def stT2(t):
        i = t % 2
        xb_ = xnb[i]
        tsl = slice(t * 128, (t + 1) * 128)
        for k2 in range(2):
            pt = g.ps[6 + k2]
            for kk in range(4):
                k = k2 * 4 + kk
                p.op("pe", lambda e, pt=pt, k=k, kk=kk: e.transpose(pt[:, kk * 128:(kk + 1) * 128], xs[:, k * 128:(k + 1) * 128], g.ident_f[:]),
                     reads=["xs", "ident_f"], writes=["ps%d" % (6 + k2)])
            for kk in range(4):
                k = k2 * 4 + kk
                p.op("act", lambda e, pt=pt, k=k, kk=kk: e.activation(out=xnf[:, k, :], in_=pt[:, kk * 128:(kk + 1) * 128], func=AF.Identity,
                                                                    scale=g.scale2T[:, k, b:b + 1], bias=g.modT[:, 24 + k, b:b + 1]),
                     reads=["ps%d" % (6 + k2)], writes=["xnf"])
        p.op("pool", lambda e: e.tensor_copy(out=xb_[:], in_=xnf[:]), reads=["xnf"], writes=["xnb%d" % i])
        p.dma("act", g.xn2T[b].rearrange("(k p) t -> p k t", p=128)[:, :, tsl], xb_[:], reads=["xnb%d" % i], writes=["scr2"])
        pr = g.ps[3]
        for k in range(8):
            p.op("pe", lambda e, k=k: e.matmul(pr[:, 0:64], lhsT=xnf[:, k, :], rhs=rw[:, k, :], start=(k == 0), stop=(k == 7)), reads=["xnf", "rw"], writes=["ps3a"])
        p.op("act", lambda e: e.activation(out=sc2[i][:], in_=pr[:, 0:64], func=AF.Sigmoid), reads=["ps3a"], writes=["sc%d" % i])
        if "dbg_sc" in dbg:
            p.dma("act", g.dbg_sc[tsl, :], sc2[i][:], reads=["sc%d" % i], writes=["dbgsc"])

    def stT3(t):
        i = t % 2
        sc_ = sc2[i]
        sk = "sc%d" % i
        cT_ = cmT[i]
        tsl = slice(t * 128, (t + 1) * 128)
        p.op("dve", lambda e: e.tensor_tensor(out=sl[:], in0=sc_[:], in1=rb[:], op=ALU.add), reads=[sk, "rb"], writes=["sl"])
        sl3 = sl[:].rearrange("p (a b) -> p a b", b=8)
        p.op("dve", lambda e: e.tensor_reduce(out=m1[:], in_=sl3, axis=AX.X, op=ALU.max), reads=["sl"], writes=["m1"])
        p.op("dve", lambda e: e.tensor_tensor(out=eq[:].rearrange("p (a b) -> p a b", b=8), in0=sl3, in1=m1[:].unsqueeze(2).to_broadcast([128, 8, 8]), op=ALU.is_equal),
             reads=["sl", "m1"], writes=["eq"])
        p.op("dve", lambda e: e.scalar_tensor_tensor(out=sl2[:], in0=eq[:], scalar=-1.0e9, in1=sl[:], op0=ALU.mult, op1=ALU.add), reads=["eq", "sl"], writes=["sl2"])
        p.op("dve", lambda e: e.tensor_reduce(out=m2[:], in_=sl2[:].rearrange("p (a b) -> p a b", b=8), axis=AX.X, op=ALU.max), reads=["sl2"], writes=["m2"])
        p.op("dve", lambda e: e.tensor_tensor(out=gs[:], in0=m1[:], in1=m2[:], op=ALU.add), reads=["m1", "m2"], writes=["gs"])
        p.op("dve", lambda e: e.max(out=mx[:], in_=gs[:]), reads=["gs"], writes=["mx"])
        p.op("dve", lambda e: e.tensor_scalar(out=gm[:], in0=gs[:], scalar1=mx[:, 3:4], scalar2=None, op0=ALU.is_ge), reads=["gs", "mx"], writes=["gm"])
        p.op("dve", lambda e: e.scalar_tensor_tensor(out=sl2[:].rearrange("p (a b) -> p a b", b=8), in0=sl3, scalar=10.0,
                                                     in1=gm[:].unsqueeze(2).to_broadcast([128, 8, 8]), op0=ALU.add, op1=ALU.mult),
             reads=["sl", "gm"], writes=["sl2"])
        p.op("dve", lambda e: e.max(out=mx[:], in_=sl2[:]), reads=["sl2"], writes=["mx"])
        p.op("dve", lambda e: e.tensor_scalar(out=eq[:], in0=sl2[:], scalar1=mx[:, 7:8], scalar2=None, op0=ALU.is_ge), reads=["sl2", "mx"], writes=["eq"])
        p.op("dve", lambda e: e.tensor_tensor(out=cmb[:], in0=sc_[:], in1=eq[:], op=ALU.mult), reads=[sk, "eq"], writes=["cmb"])
        p.op("dve", lambda e: e.tensor_reduce(out=stat[:, 4:5], in_=cmb[:], axis=AX.X, op=ALU.add), reads=["cmb"], writes=["stat4"])
        p.op("dve", lambda e: e.reciprocal(out=stat[:, 5:6], in_=stat[:, 4:5]), reads=["stat4"], writes=["stat5"])
        p.op("dve", lambda e: e.tensor_scalar(out=cmb[:], in0=cmb[:], scalar1=stat[:, 5:6], scalar2=2.5, op0=ALU.mult, op1=ALU.mult), reads=["cmb", "stat5"], writes=["cmb"])
        p.op("pe", lambda e: e.transpose(g.ps[3][0:64, 128:256], cmb[:], g.ident_f[:]), reads=["cmb", "ident_f"], writes=["ps3b"])
        p.op("act", lambda e: e.activation(out=cT_[:], in_=g.ps[3][0:64, 128:256], func=AF.Copy), reads=["ps3b"], writes=["cmT%d" % i])
        p.dma("act", g.combT[b][:, tsl], cT_[:], reads=["cmT%d" % i], writes=["scr3"])

    stG(0)
    stY(0)
    stT1(0)
    for t in range(NTILE):
        if t + 1 < NTILE:
            if (t + 1) % 4 == 0:
                stG((t + 1) // 4)
            stY(t + 1)
        stT2(t)
        if t + 1 < NTILE:
            stT1(t + 1)
        stT3(t)
    p.pop()


def phase_moe(g, b, dbg=()):
    p = g.p
    TGM = 2048
    NT = TGM // 128
    SG = 256
    groups = [[2 * i, 2 * i + 1] for i in range(32)] + [[64]]
    if "moe1" in dbg:
        groups = groups[:1] + groups[-1:]
    for tgi in range(L // TGM):
        p.push()
        t0 = tgi * TGM
        xn = p.sbuf("xn", [128, 8, TGM], BF16)
        yacc = p.sbuf("yacc", [128, NT, D], F32)
        cT = p.sbuf("cT", [64, TGM], BF16)
        sel = p.sbuf("sel", [64, 64, 128], BF16)
        p.push()
        wst1 = p.sbuf("wst1", [128, 8, 256], F32)
        wst3 = p.sbuf("wst3", [128, 8, 256], F32)
        wst2 = p.sbuf("wst2", [128, 2, D], F32)
        w1b = [p.sbuf("w1b%d" % i, [128, 8, 256], BF16) for i in range(4)]
        w3b = [p.sbuf("w3b%d" % i, [128, 8, 256], BF16) for i in range(4)]
        w2b = [p.sbuf("w2b%d" % i, [128, 2, D], BF16) for i in range(4)]
        sl = [p.sbuf("sl%d" % i, [128, 2, SG], F32) for i in range(2)]
        aT = [p.sbuf("aT%d" % i, [128, 2, SG], BF16) for i in range(2)]
        p.dma("sp", xn[:], g.xn2T[b].rearrange("(k p) t -> p k t", p=128)[:, :, t0:t0 + TGM], writes=["xn"])
        p.dma("sp", cT[:], g.combT[b][:, t0:t0 + TGM], writes=["cT"])
        for i in range(4):
            self_f = wst1[0:64, :, :].rearrange("p a b -> p (a b)")
            p.dma("sp", self_f, g.sel[:, i * 16:(i + 1) * 16, :].rearrange("p a b -> p (a b)"), writes=["wst1"])
            p.op("dve", lambda e, i=i, self_f=self_f: e.tensor_copy(out=sel[:, i * 16:(i + 1) * 16, :].rearrange("p a b -> p (a b)"), in_=self_f), reads=["wst1"], writes=["sel"])

        def load(gi):
            for j, e_ in enumerate(groups[gi]):
                s = (2 * gi + j) % 4
                if e_ < 64:
                    s1 = g.exp_w1[e_].rearrange("(k p) f -> p k f", p=128)
                    s3 = g.exp_w3[e_].rearrange("(k p) f -> p k f", p=128)
                    s2 = g.exp_w2[e_].rearrange("(k p) d -> p k d", p=128)
                else:
                    s1 = g.sh_w1.rearrange("(k p) f -> p k f", p=128)
                    s3 = g.sh_w3.rearrange("(k p) f -> p k f", p=128)
                    s2 = g.sh_w2.rearrange("(k p) d -> p k d", p=128)
                p.dma("sp", wst1[:], s1, writes=["wst1"])
                p.dma("sp", wst3[:], s3, writes=["wst3"])
                p.dma("sp", wst2[:], s2, writes=["wst2"])
                if gi < 2:
                    p.op("act", lambda e, s=s: e.activation(out=w1b[s][:], in_=wst1[:], func=AF.Copy), reads=["wst1"], writes=["w1b%d" % s])
                    p.op("dve", lambda e, s=s: e.tensor_copy(out=w3b[s][:], in_=wst3[:]), reads=["wst3"], writes=["w3b%d" % s])
                else:
                    p.op("pool", lambda e, s=s: e.tensor_copy(out=w1b[s][:], in_=wst1[:]), reads=["wst1"], writes=["w1b%d" % s])
                    p.op("pool", lambda e, s=s: e.tensor_copy(out=w3b[s][:], in_=wst3[:]), reads=["wst3"], writes=["w3b%d" % s])
                p.op("pool", lambda e, s=s: e.tensor_copy(out=w2b[s][:], in_=wst2[:]), reads=["wst2"], writes=["w2b%d" % s])

        ycnt = [0]
        sl4 = [sl[0], sl[1], p.sbuf("sl2", [128, 2, SG], F32), p.sbuf("sl3", [128, 2, SG], F32)]
        aT4 = [aT[0], aT[1], p.sbuf("aT2", [128, 2, SG], BF16), p.sbuf("aT3", [128, 2, SG], BF16)]

        def stepH(gi, sg_):
            exps = groups[gi]
            par = sg_ % 2
            ts = slice(sg_ * SG, (sg_ + 1) * SG)
            for j, e_ in enumerate(exps):
                s = (2 * gi + j) % 4
                jj = 2 * par + j
                ph1, ph3 = g.ps[2 * j], g.ps[2 * j + 1]
                k1, k3 = "ps%d" % (2 * j), "ps%d" % (2 * j + 1)
                pc = g.ps[4][:, j * SG:(j + 1) * SG]
                for fc in range(2):
                    fs = slice(fc * 128, (fc + 1) * 128)
                    for k in range(8):
                        p.op("pe", lambda e, fc=fc, fs=fs, k=k, s=s, ph1=ph1: e.matmul(ph1[:, fc * SG:(fc + 1) * SG], lhsT=w1b[s][:, k, fs], rhs=xn[:, k, ts],
                                                                                   start=(k == 0), stop=(k == 7)), reads=["w1b%d" % s, "xn"], writes=[k1])
                for fc in range(2):
                    fs = slice(fc * 128, (fc + 1) * 128)
                    for k in range(8):
                        p.op("pe", lambda e, fc=fc, fs=fs, k=k, s=s, ph3=ph3: e.matmul(ph3[:, fc * SG:(fc + 1) * SG], lhsT=w3b[s][:, k, fs], rhs=xn[:, k, ts],
                                                                                   start=(k == 0), stop=(k == 7)), reads=["w3b%d" % s, "xn"], writes=[k3])
                if e_ < 64:
                    p.op("pe", lambda e, e_=e_, pc=pc: e.matmul(pc, lhsT=sel[:, e_, :], rhs=cT[:, ts], start=True, stop=True), reads=["sel", "cT"], writes=["ps4_%d" % j])
                slv = sl4[jj][:].rearrange("p a b -> p (a b)")
                p.op("act", lambda e, slv=slv, ph1=ph1: e.activation(out=slv, in_=ph1[:], func=AF.Silu), reads=[k1], writes=["sl%d" % jj])
                if e_ < 64:
                    p.op("dve", lambda e, slv=slv, ph3=ph3: e.tensor_tensor(out=slv, in0=ph3[:], in1=slv, op=ALU.mult), reads=[k3, "sl%d" % jj], writes=["sl%d" % jj])
                    p.op("dve", lambda e, jj=jj, pc=pc: e.tensor_tensor(out=aT4[jj][:], in0=sl4[jj][:], in1=pc.unsqueeze(1).to_broadcast([128, 2, SG]), op=ALU.mult),
                         reads=["ps4_%d" % j, "sl%d" % jj], writes=["aT%d" % jj])
                else:
                    p.op("dve", lambda e, jj=jj, slv=slv, ph3=ph3: e.tensor_tensor(out=aT4[jj][:].rearrange("p a b -> p (a b)"), in0=ph3[:], in1=slv, op=ALU.mult),
                         reads=[k3, "sl%d" % jj], writes=["aT%d" % jj])

        def stepY(gi, sg_):
            exps = groups[gi]
            par = sg_ % 2
            for tt in range(SG // 128):
                tl = sg_ * (SG // 128) + tt
                for hf in range(2):
                    ycnt[0] += 1
                    bank = 5 + ycnt[0] % 3
                    py = g.ps[bank]
                    nmm = 2 * len(exps)
                    i = 0
                    for j, e_ in enumerate(exps):
                        s = (2 * gi + j) % 4
                        jj = 2 * par + j
                        for fc in range(2):
                            p.op("pe", lambda e, py=py, fc=fc, tt=tt, hf=hf, jj=jj, s=s, i=i, nmm=nmm: e.matmul(
                                py[:], lhsT=aT4[jj][:, fc, tt * 128:(tt + 1) * 128], rhs=w2b[s][:, fc, hf * 512:(hf + 1) * 512],
                                start=(i == 0), stop=(i == nmm - 1)), reads=["aT%d" % jj, "w2b%d" % s], writes=["ps%d" % bank])
                            i += 1
                    dst = yacc[:, tl, hf * 512:(hf + 1) * 512]
                    yk = "yacc%d_%d" % (tl, hf)
                    if gi == 0:
                        p.op("act", lambda e, py=py, dst=dst: e.activation(out=dst, in_=py[:], func=AF.Copy), reads=["ps%d" % bank], writes=[yk])
                    else:
                        p.op("dve", lambda e, py=py, dst=dst: e.tensor_tensor(out=dst, in0=py[:], in1=dst, op=ALU.add), reads=["ps%d" % bank, yk], writes=[yk])

        steps = [(gi, sg_) for gi in range(len(groups)) for sg_ in range(TGM // SG)]
        load(0)
        if len(groups) > 1:
            load(1)
        stepH(*steps[0])
        for si, (gi, sg_) in enumerate(steps):
            if si + 1 < len(steps):
                stepH(*steps[si + 1])
            stepY(gi, sg_)
            if sg_ == TGM // SG - 1 and gi + 2 < len(groups):
                load(gi + 2)
        p.pop()
        p.push()
        g2b = p.sbuf("g2b", [128, D], F32)
        fnw = p.sbuf("fnw", [128, D], F32)
        x1t = [p.sbuf("x1t%d" % i, [128, D], F32) for i in range(2)]
        tmp = [p.sbuf("tmp%d" % i, [128, D], F32) for i in range(2)]
        st = p.sbuf("st", [128, 4], F32)
        p.dma("sp", g2b[:], g.gbc_d[:, (2 + b) * D:(3 + b) * D], writes=["g2b"])
        p.dma("sp", fnw[:], g.final_w[0:1, :].partition_broadcast(128), writes=["fnw"])
        for tl in range(NT):
            i = tl % 2
            rows = slice(t0 + tl * 128, t0 + (tl + 1) * 128)
            p.dma("sp", x1t[i][:], g.x1d[b][rows, :], writes=["x1t%d" % i])
            p.op("dve", lambda e, i=i, tl=tl: e.tensor_tensor(out=tmp[i][:], in0=yacc[:, tl, :], in1=g2b[:], op=ALU.mult), reads=["g2b"], writes=["tmp%d" % i])
            p.op("pool", lambda e, i=i: e.tensor_tensor(out=tmp[i][:], in0=tmp[i][:], in1=x1t[i][:], op=ALU.add), reads=["tmp%d" % i, "x1t%d" % i], writes=["tmp%d" % i])
            p.op("act", lambda e, i=i: e.activation(out=x1t[i][:], in_=tmp[i][:], func=AF.Square, accum_out=st[:, 0:1]), reads=["tmp%d" % i], writes=["x1t%d" % i, "st"])
            p.op("act", lambda e: e.activation(out=st[:, 1:2], in_=st[:, 0:1], func=AF.Ln, scale=1.0 / D, bias=EPS), reads=["st"], writes=["st"])
            p.op("act", lambda e: e.activation(out=st[:, 2:3], in_=st[:, 1:2], func=AF.Exp, scale=-0.5), reads=["st"], writes=["st"])
            p.op("dve", lambda e, i=i: e.scalar_tensor_tensor(out=x1t[i][:], in0=tmp[i][:], scalar=st[:, 2:3], in1=fnw[:], op0=ALU.mult, op1=ALU.mult),
                 reads=["tmp%d" % i, "st", "fnw"], writes=["x1t%d" % i])
            p.dma("act", g.out[b][rows, :], x1t[i][:], reads=["x1t%d" % i], writes=["out"])
        p.pop()
        p.pop()
    return


def make_in_maps(inputs):
    x = np.asarray(inputs["x"], np.float32)
    c = np.asarray(inputs["c"], np.float32)
    ctx = np.asarray(inputs["ctx"], np.float32)
    c_ctx = np.asarray(inputs["c_ctx"], np.float32)
    hc = host_consts()
    shared = {
        "ada_w": np.ascontiguousarray(inputs["ada_w"][0]),
        "ada_bT": fm(inputs["ada_b"][0], 48),
        "ada_b": np.ascontiguousarray(inputs["ada_b"][0].reshape(1, -1)),
        "norm1_wT": fm(inputs["norm1_w"][0], 8),
        "norm2_wT": fm(inputs["norm2_w"][0], 8),
        "w_in": np.ascontiguousarray(inputs["w_in"][0]),
        "hy_conv_wT": np.ascontiguousarray(np.asarray(inputs["hy_conv_w"][0], np.float32).reshape(3, 24, 128).transpose(2, 1, 0)),
        "hy_conv_bT": fm(inputs["hy_conv_b"][0], 24),
        "ident": hc["ident"],
        "a_w2e": hc_a_w2e(inputs["gla_a_w2"][0]),
        "a_bT": np.ascontiguousarray(np.asarray(inputs["gla_a_b"][0], np.float32).reshape(2, 4, 128).transpose(2, 0, 1)),
        "gnwT": np.ascontiguousarray(np.asarray(inputs["gla_norm_w"][0], np.float32).reshape(4, 2, 128).transpose(2, 0, 1)),
        "maskf": hc["maskf"],
        "maskb": hc["maskb"],
        "proj_hy": np.ascontiguousarray(inputs["proj_hy"][0]), "proj_gla": np.ascontiguousarray(inputs["proj_gla"][0]),
        "w_out": np.ascontiguousarray(inputs["w_out"][0]), "router_w": np.ascontiguousarray(inputs["router_w"][0]),
        "router_b": np.ascontiguousarray(inputs["router_bias"][0].reshape(1, 64)),
        "final_w": np.ascontiguousarray(np.asarray(inputs["final_norm_w"], np.float32).reshape(1, D)),
        "exp_w1": np.ascontiguousarray(inputs["exp_w1"][0]), "exp_w3": np.ascontiguousarray(inputs["exp_w3"][0]),
        "exp_w2": np.ascontiguousarray(inputs["exp_w2"][0]),
        "sh_w1": np.ascontiguousarray(inputs["sh_w1"][0]), "sh_w3": np.ascontiguousarray(inputs["sh_w3"][0]),
        "sh_w2": np.ascontiguousarray(inputs["sh_w2"][0]),
        "sel": hc["sel"],
        "zext": hc["zext"], "text": hc["text"], "deltaT": hc["deltaT"],
        "hy_w1": np.ascontiguousarray(inputs["hy_w1"][0]), "hy_w2": np.ascontiguousarray(inputs["hy_w2"][0]),
        "hy_w3": np.ascontiguousarray(inputs["hy_w3"][0]),
        "hy_bf": np.ascontiguousarray(np.stack([inputs["hy_b1"][0], inputs["hy_b2"][0], inputs["hy_freq"][0][0], inputs["hy_freq"][0][1]], axis=1).astype(np.float32)),
        "hy_biasT": np.ascontiguousarray(np.asarray(inputs["hy_bias"][0], np.float32).reshape(2, 16, 64).transpose(2, 1, 0)),
        "tF64": hc["tF64"], "tG": hc["tG"], "tGp": hc["tGp"], "tC3": hc["tC3"],
    }
    maps = []
    for core in range(8):
        b0 = core * NB
        c3 = np.zeros((4, D), np.float32)
        c3[0] = c[b0]
        c3[1] = c[b0 + 1]
        c3[2] = c_ctx
        m = dict(shared)
        m["x"] = np.ascontiguousarray(x[b0:b0 + NB])
        m["ctx"] = np.ascontiguousarray(ctx[b0:b0 + NB])
        m["c3T"] = np.ascontiguousarray(c3.reshape(4, 8, 128).transpose(2, 1, 0))
        maps.append(m)
    return maps


def kernel(**inputs):
    nc = build_program()
    maps = make_in_maps(inputs)
    res = run_bass_kernel_spmd(nc, maps, core_ids=list(range(8)))
    return np.concatenate([np.asarray(r["out"]) for r in res.results], axis=0)
```
